# Optimizing a Trainium2 kernel written in Bass

```python
import jax, jax.numpy as jnp
from jax import lax
import numpy as np

D_MODEL = 1024
BATCH = 16
SEQ = 4096
DEPTH = 1

GRID_W = 64
WIN_H_MAX = 8
WIN_W = 16
ATTN_HEADS = 8
ATTN_HEAD_DIM = 64
ATTN_WIDTH = ATTN_HEADS * ATTN_HEAD_DIM
DN_HEADS = 4
DN_HEAD_DIM = 128
DN_WIDTH = DN_HEADS * DN_HEAD_DIM
DN_CONV = 5
DN_CHUNK = 64
D_MIX = ATTN_WIDTH + DN_WIDTH
IN_COLS = 3 * ATTN_WIDTH + 4 * DN_WIDTH + 4 * DN_HEADS
N_GROUPS = 8
EXPERTS_PER_GROUP = 8
N_EXPERTS = N_GROUPS * EXPERTS_PER_GROUP
TOP_K_IN_GROUP = 2
D_EXPERT = D_MODEL // 4
MOE_BLOCK = 256
EPS = 1e-6

kernel_name = 'hybrid_na_gdn_hmoe_block'


def rms_norm(x, g):
    xf = x.astype(jnp.float32)
    y = xf * lax.rsqrt(jnp.mean(xf * xf, axis=-1, keepdims=True) + EPS)
    return (y * g.astype(jnp.float32)).astype(x.dtype)


def l2_norm(x):
    xf = x.astype(jnp.float32)
    return xf * lax.rsqrt(jnp.sum(xf * xf, axis=-1, keepdims=True) + EPS)


def neighbourhood_attention(q, k, v, rpb):
    b_, s_, h_, dh = q.shape
    rows = s_ // GRID_W
    kh = min(WIN_H_MAX, rows)
    to_grid = lambda t: t.reshape(b_, rows, GRID_W, h_, dh).transpose(1, 0, 3, 2, 4)
    qg, kg, vg = to_grid(q), to_grid(k), to_grid(v)
    cols = jnp.arange(GRID_W)
    col_start = jnp.clip(cols - WIN_W // 2, 0, GRID_W - WIN_W)
    col_idx = col_start[:, None] + jnp.arange(WIN_W)[None, :]
    col_off = col_idx - cols[:, None] + (WIN_W - 1)
    scale = dh ** -0.5

    def row_block(args):
        r, q_row = args
        r0 = jnp.clip(r - kh // 2, 0, rows - kh)
        k_band = lax.dynamic_slice_in_dim(kg, r0, kh, axis=0)
        v_band = lax.dynamic_slice_in_dim(vg, r0, kh, axis=0)
        k_win = k_band[:, :, :, col_idx]
        v_win = v_band[:, :, :, col_idx]
        row_off = r0 + jnp.arange(kh) - r + (WIN_H_MAX - 1)
        bias = rpb[:, row_off[None, :, None], col_off[:, None, :]]
        logits = jnp.einsum('bhqd,rbhqcd->bhqrc', q_row, k_win).astype(jnp.float32) * scale
        logits = logits + bias.astype(jnp.float32)[None]
        p = jax.nn.softmax(logits.reshape(b_, h_, GRID_W, kh * WIN_W), axis=-1)
        p = p.reshape(b_, h_, GRID_W, kh, WIN_W).astype(v.dtype)
        return jnp.einsum('bhqrc,rbhqce->bqhe', p, v_win)

    out = lax.map(row_block, (jnp.arange(rows), qg))
    return out.transpose(1, 0, 2, 3, 4).reshape(b_, s_, h_ * dh)


def gated_delta_rule(q, k, v, g, beta):
    f32 = jnp.float32
    bn, hn, t_, dk = q.shape
    dv = v.shape[-1]
    c = DN_CHUNK
    nc = t_ // c
    q = q.astype(f32).reshape(bn, hn, nc, c, dk)
    k = k.astype(f32).reshape(bn, hn, nc, c, dk)
    v = v.astype(f32).reshape(bn, hn, nc, c, dv)
    beta = beta.astype(f32).reshape(bn, hn, nc, c)
    gc = jnp.cumsum(g.astype(f32).reshape(bn, hn, nc, c), axis=-1)
    incl = jnp.tril(jnp.ones((c, c), bool))
    strict = jnp.tril(jnp.ones((c, c), bool), -1)
    diff = gc[..., :, None] - gc[..., None, :]
    decay = jnp.where(incl, jnp.exp(jnp.where(incl, diff, 0.0)), 0.0)
    kb = k * beta[..., None]
    lower = jnp.where(strict, jnp.einsum('bhnid,bhnjd->bhnij', kb, k) * decay, 0.0)
    a_mat = jnp.eye(c, dtype=f32) + lower
    rhs = jnp.concatenate([v * beta[..., None], kb * jnp.exp(gc)[..., None]], axis=-1)
    sol = lax.linalg.triangular_solve(a_mat, rhs, left_side=True, lower=True, unit_diagonal=True)
    u, w = sol[..., :dv], sol[..., dv:]
    qk = jnp.einsum('bhnid,bhnjd->bhnij', q, k) * decay

    def step(state, inp):
        q_c, k_c, u_c, w_c, g_c, qk_c = inp
        v_new = u_c - jnp.einsum('bhck,bhkv->bhcv', w_c, state)
        o_c = (jnp.einsum('bhck,bhkv->bhcv', q_c * jnp.exp(g_c)[..., None], state)
               + jnp.einsum('bhij,bhjv->bhiv', qk_c, v_new))
        g_last = g_c[..., -1:]
        state = (state * jnp.exp(g_last)[..., None]
                 + jnp.einsum('bhck,bhcv->bhkv', k_c * jnp.exp(g_last - g_c)[..., None], v_new))
        return state, o_c

    xs = tuple(jnp.moveaxis(t, 2, 0) for t in (q, k, u, w, gc, qk))
    state0 = jnp.zeros((bn, hn, dk, dv), f32)
    _, o = lax.scan(step, state0, xs)
    return jnp.moveaxis(o, 0, 2).reshape(bn, hn, t_, dv)


def bidirectional_gated_deltanet(qkv_raw, z, a, b, conv_w, a_log, dt_bias, out_norm_g):
    b_, s_, _ = qkv_raw.shape
    qkv = lax.conv_general_dilated(qkv_raw, conv_w[:, None, :].astype(qkv_raw.dtype),
                                   window_strides=(1,), padding=[(DN_CONV // 2, DN_CONV // 2)],
                                   dimension_numbers=('NWC', 'WIO', 'NWC'),
                                   feature_group_count=3 * DN_WIDTH)
    qkv = jax.nn.silu(qkv)
    q, k, v = jnp.split(qkv.reshape(b_, s_, 3, DN_HEADS, DN_HEAD_DIM), 3, axis=2)
    q = l2_norm(q[:, :, 0]) * (DN_HEAD_DIM ** -0.5)
    k = l2_norm(k[:, :, 0])
    v = v[:, :, 0].astype(jnp.float32)
    g = -jnp.exp(a_log.astype(jnp.float32)) * jax.nn.softplus(a.astype(jnp.float32) + dt_bias.astype(jnp.float32))
    beta = jax.nn.sigmoid(b.astype(jnp.float32))
    bht = lambda t: t.transpose(0, 2, 1, 3)
    both = lambda t: jnp.concatenate([t, jnp.flip(t, axis=2)], axis=1)
    q2, k2, v2 = both(bht(q)), both(bht(k)), both(bht(v))
    g2 = jnp.concatenate([g[:, :, 0].transpose(0, 2, 1), jnp.flip(g[:, :, 1].transpose(0, 2, 1), axis=-1)], axis=1)
    beta2 = jnp.concatenate([beta[:, :, 0].transpose(0, 2, 1), jnp.flip(beta[:, :, 1].transpose(0, 2, 1), axis=-1)], axis=1)
    o2 = gated_delta_rule(q2, k2, v2, g2, beta2)
    o = o2[:, :DN_HEADS] + jnp.flip(o2[:, DN_HEADS:], axis=2)
    o = o.transpose(0, 2, 1, 3)
    zf = z.reshape(b_, s_, DN_HEADS, DN_HEAD_DIM).astype(jnp.float32)
    o = rms_norm(o, out_norm_g) * jax.nn.silu(zf)
    return o.reshape(b_, s_, DN_WIDTH).astype(qkv_raw.dtype)


def hierarchical_moe(h, wg_r, bg_r, we_r, be_r, w_gate, w_up, w_down):
    b_, s_, d_ = h.shape
    n = b_ * s_
    hf = h.reshape(n, d_)
    group_logits = (hf @ wg_r + bg_r).astype(jnp.float32)
    group_prob = jax.nn.softmax(group_logits, axis=-1)
    gsel = jnp.argmax(group_logits, axis=-1)
    gp = jnp.take_along_axis(group_prob, gsel[:, None], axis=-1)
    exp_logits = (hf @ we_r + be_r).astype(jnp.float32).reshape(n, N_GROUPS, EXPERTS_PER_GROUP)
    in_group = jnp.take_along_axis(exp_logits, gsel[:, None, None], axis=1)[:, 0]
    top_val, top_idx = lax.top_k(in_group, TOP_K_IN_GROUP)
    gates = gp * jax.nn.softmax(top_val, axis=-1)
    eid = (gsel[:, None] * EXPERTS_PER_GROUP + top_idx).reshape(-1).astype(jnp.int32)
    gate_flat = gates.reshape(-1)
    tok_flat = jnp.repeat(jnp.arange(n, dtype=jnp.int32), TOP_K_IN_GROUP)
    nk = n * TOP_K_IN_GROUP
    p_rows = -(-nk // MOE_BLOCK) * MOE_BLOCK + N_EXPERTS * MOE_BLOCK
    nb = p_rows // MOE_BLOCK
    order = jnp.argsort(eid)
    e_sorted = eid[order]
    counts = jnp.bincount(eid, length=N_EXPERTS)
    starts = jnp.cumsum(counts) - counts
    padded = -(-counts // MOE_BLOCK) * MOE_BLOCK
    pends = jnp.cumsum(padded)
    pstarts = pends - padded
    dest = pstarts[e_sorted] + (jnp.arange(nk) - starts[e_sorted])
    buf_tok = jnp.full((p_rows,), n, jnp.int32).at[dest].set(tok_flat[order])
    buf_gate = jnp.zeros((p_rows,), jnp.float32).at[dest].set(gate_flat[order])
    block_expert = jnp.clip(jnp.searchsorted(pends, jnp.arange(nb) * MOE_BLOCK, side='right'),
                            0, N_EXPERTS - 1).astype(jnp.int32)
    h_pad = jnp.concatenate([hf, jnp.zeros((1, d_), hf.dtype)], axis=0)

    def expert_block(args):
        tok, e = args
        xb = h_pad[tok]
        hid = jax.nn.silu(xb @ w_gate[e]) * (xb @ w_up[e])
        return hid @ w_down[e]

    out = lax.map(expert_block, (buf_tok.reshape(nb, MOE_BLOCK), block_expert))
    out = (out.reshape(p_rows, d_).astype(jnp.float32) * buf_gate[:, None]).astype(h.dtype)
    y = jnp.zeros((n + 1, d_), h.dtype).at[buf_tok].add(out)[:n]
    return y.reshape(b_, s_, d_)


def hybrid_layer(x, ln1_g, w_in, q_norm_g, k_norm_g, rpb, attn_out_g, conv_w, a_log, dt_bias,
                 dn_out_g, w_out, ln2_g, wg_r, bg_r, we_r, be_r, w_gate, w_up, w_down):
    b_, s_, _ = x.shape
    h = rms_norm(x, ln1_g)
    proj = h @ w_in
    a3 = 3 * ATTN_WIDTH
    aq, ak, av, dqkv, dz, dab = jnp.split(
        proj, [ATTN_WIDTH, 2 * ATTN_WIDTH, a3, a3 + 3 * DN_WIDTH, a3 + 4 * DN_WIDTH], axis=-1)
    hs = lambda t: t.reshape(b_, s_, ATTN_HEADS, ATTN_HEAD_DIM)
    aq = rms_norm(hs(aq), q_norm_g)
    ak = rms_norm(hs(ak), k_norm_g)
    attn = rms_norm(neighbourhood_attention(aq, ak, hs(av), rpb), attn_out_g)
    dab = dab.reshape(b_, s_, 2, 2, DN_HEADS)
    dn = bidirectional_gated_deltanet(dqkv, dz, dab[:, :, 0], dab[:, :, 1], conv_w, a_log,
                                      dt_bias, dn_out_g)
    x = x + jnp.concatenate([attn, dn], axis=-1) @ w_out
    x = x + hierarchical_moe(rms_norm(x, ln2_g), wg_r, bg_r, we_r, be_r, w_gate, w_up, w_down)
    return x


def setup_inputs(seed: int = 0) -> dict:
    key = jax.random.key(seed)
    ks = jax.random.split(key, 24)
    f32 = jnp.float32
    nrm = lambda k, shape, s: jax.random.normal(k, shape, f32) * s
    gain = lambda k, shape: 1.0 + 0.01 * jax.random.normal(k, shape, f32)
    L = DEPTH
    dt = jnp.exp(jax.random.uniform(ks[9], (L, 2, DN_HEADS), f32, np.log(1e-3), np.log(1e-1)))
    return {
        'x': nrm(ks[0], (BATCH, SEQ, D_MODEL), 1.0),
        'ln1_g': gain(ks[1], (L, D_MODEL)),
        'w_in': nrm(ks[2], (L, D_MODEL, IN_COLS), D_MODEL ** -0.5),
        'attn_q_norm_g': gain(ks[3], (L, ATTN_HEAD_DIM)),
        'attn_k_norm_g': gain(ks[4], (L, ATTN_HEAD_DIM)),
        'attn_rpb': nrm(ks[5], (L, ATTN_HEADS, 2 * WIN_H_MAX - 1, 2 * WIN_W - 1), 0.1),
        'attn_out_norm_g': gain(ks[6], (L, ATTN_WIDTH)),
        'dn_conv_w': nrm(ks[7], (L, DN_CONV, 3 * DN_WIDTH), DN_CONV ** -0.5),
        'dn_a_log': jnp.log(jax.random.uniform(ks[8], (L, 2, DN_HEADS), f32, 1.0, 16.0)),
        'dn_dt_bias': dt + jnp.log(-jnp.expm1(-dt)),
        'dn_out_norm_g': gain(ks[10], (L, DN_HEAD_DIM)),
        'w_out': nrm(ks[11], (L, D_MIX, D_MODEL), D_MIX ** -0.5),
        'ln2_g': gain(ks[12], (L, D_MODEL)),
        'router_group_w': nrm(ks[13], (L, D_MODEL, N_GROUPS), D_MODEL ** -0.5),
        'router_group_b': nrm(ks[14], (L, N_GROUPS), 0.01),
        'router_expert_w': nrm(ks[15], (L, D_MODEL, N_EXPERTS), D_MODEL ** -0.5),
        'router_expert_b': nrm(ks[16], (L, N_EXPERTS), 0.01),
        'expert_w_gate': nrm(ks[17], (L, N_EXPERTS, D_MODEL, D_EXPERT), D_MODEL ** -0.5),
        'expert_w_up': nrm(ks[18], (L, N_EXPERTS, D_MODEL, D_EXPERT), D_MODEL ** -0.5),
        'expert_w_down': nrm(ks[19], (L, N_EXPERTS, D_EXPERT, D_MODEL), D_EXPERT ** -0.5),
    }


def reference(x, ln1_g, w_in, attn_q_norm_g, attn_k_norm_g, attn_rpb, attn_out_norm_g, dn_conv_w,
              dn_a_log, dn_dt_bias, dn_out_norm_g, w_out, ln2_g, router_group_w, router_group_b,
              router_expert_w, router_expert_b, expert_w_gate, expert_w_up, expert_w_down):
    for l in range(DEPTH):
        x = hybrid_layer(x, ln1_g[l], w_in[l], attn_q_norm_g[l], attn_k_norm_g[l], attn_rpb[l],
                         attn_out_norm_g[l], dn_conv_w[l], dn_a_log[l], dn_dt_bias[l],
                         dn_out_norm_g[l], w_out[l], ln2_g[l], router_group_w[l], router_group_b[l],
                         router_expert_w[l], router_expert_b[l], expert_w_gate[l], expert_w_up[l],
                         expert_w_down[l])
    return x
```

```python
from contextlib import ExitStack
import numpy as np
import concourse.bass as bass
import concourse.mybir as mybir
from concourse.bass_utils import run_bass_kernel_spmd

F32 = mybir.dt.float32
BF16 = mybir.dt.bfloat16
AF = mybir.ActivationFunctionType
ALU = mybir.AluOpType
AX = mybir.AxisListType

D = 1024
T = 4096
INC = 3600
EPS = 1e-6
ENGS = ("pe", "act", "dve", "pool", "sp")


class Buf:
    __slots__ = ("name", "last_w", "reads", "sem")

    def __init__(self, name):
        self.name = name
        self.last_w = None
        self.reads = []
        self.sem = None


class Sched:
    def __init__(self, nc, es, n_hw_sems=64, n_sw_sems=16):
        self.nc = nc
        self.sems = {e: es.enter_context(nc.semaphore("s_" + e)) for e in ENGS}
        for i in range(n_hw_sems):
            self.sems[("hw", i)] = es.enter_context(nc.semaphore("s_hw%d" % i))
        for i in range(n_sw_sems):
            self.sems[("sw", i)] = es.enter_context(nc.semaphore("s_sw%d" % i))
        self.n_sems = {"hw": n_hw_sems, "sw": n_sw_sems}
        self._nd = {"hw": 0, "sw": 0}
        self.ops = {e: [] for e in ENGS}
        self.tick = {e: 0 for e in ENGS}
        self.known = {e: {} for e in ENGS}
        self.dma_cnt = {}

    def _deps(self, eng, reads, writes):
        deps = {}

        def add(ev):
            if ev is None:
                return
            k, c = ev
            if k == "pe" and eng == "pe":
                return
            if deps.get(k, 0) < c:
                deps[k] = c
        for b in reads:
            add(b.last_w)
            if b.name[:2] in ("ps", "pf", "pn", "po", "st", "pt", "pl", "pg", "pu", "py"):
                for r in b.reads:
                    if r[0] != eng:
                        add(r)
        for b in writes:
            add(b.last_w)
            for r in b.reads:
                add(r)
        kn = self.known[eng]
        out = []
        for k, c in deps.items():
            if kn.get(k, 0) >= c:
                continue
            kn[k] = c
            out.append((k, c))
        return out

    def _commit(self, ev, reads, writes):
        for b in reads:
            b.reads.append(ev)
            if len(b.reads) > 32:
                m = {}
                for k, c in b.reads:
                    if m.get(k, 0) < c:
                        m[k] = c
                b.reads = list(m.items())
        for b in writes:
            b.last_w = ev
            b.reads = []

    def op(self, eng, fn, reads=(), writes=(), inc=True):
        waits = self._deps(eng, reads, writes)
        if inc:
            self.tick[eng] += 1
            ev = (eng, self.tick[eng])
            self.ops[eng].append((waits, fn, (eng, 1)))
        else:
            ev = (eng, self.tick[eng] + 1)
            self.ops[eng].append((waits, fn, None))
        self._commit(ev, reads, writes)
        return ev

    def dma(self, eng, fn, reads=(), writes=(), sem_buf=None):
        if sem_buf is None:
            sem_buf = (list(writes) + list(reads))[0]
        cls = "sw" if eng == "pool" else "hw"
        if sem_buf.sem is None:
            sem_buf.sem = (cls, self._nd[cls] % self.n_sems[cls])
            self._nd[cls] += 1
        k = sem_buf.sem
        assert k[0] == cls, "buffer %s mixes software- and hardware-DGE DMAs on one semaphore" % sem_buf.name
        waits = self._deps(eng, reads, writes)
        prev = self.dma_cnt.get(k, 0)
        if prev and self.known[eng].get(k, 0) < prev:
            self.known[eng][k] = prev
            waits.append((k, prev))
        cnt = prev + 16
        self.dma_cnt[k] = cnt
        ev = (k, cnt)
        self.ops[eng].append((waits, fn, (k, 16)))
        self._commit(ev, reads, writes)
        return ev

    def barrier(self):
        for eng in ENGS:
            waits = []
            kn = self.known[eng]
            for e2 in ENGS:
                c = self.tick[e2]
                if e2 != "sp" and c and kn.get(e2, 0) < c and not (e2 == "pe" and eng == "pe"):
                    kn[e2] = c
                    waits.append((e2, c))
            for k, c in self.dma_cnt.items():
                if kn.get(k, 0) < c:
                    kn[k] = c
                    waits.append((k, c))
            if waits:
                self.ops[eng].append((waits, None, None))

    def wait_all(self, eng, bufs):
        self.ops[eng].append((self._deps(eng, bufs, bufs), None, None))

    def emit(self):
        nc, sems, ops = self.nc, self.sems, self.ops

        def run(engine, lst):
            for waits, fn, inc in lst:
                for k, c in waits:
                    engine.wait_ge(sems[k], c)
                if fn is None:
                    continue
                inst = fn(engine)
                if inc is not None:
                    inst.then_inc(sems[inc[0]], inc[1])

        with nc.Block() as block:
            @block.tensor
            def _(e):
                run(e, ops["pe"])

            @block.scalar
            def _(e):
                run(e, ops["act"])

            @block.vector
            def _(e):
                run(e, ops["dve"])

            @block.gpsimd
            def _(e):
                run(e, ops["pool"])

            @block.sync
            def _(e):
                run(e, ops["sp"])
        self.ops = {e: [] for e in ENGS}


def _attn_tables(rpb):
    kl = np.arange(128)
    krl, kc = kl // 64, kl % 64
    ql = np.arange(128)
    qrl, qc = ql // 64, ql % 64
    bias9 = np.zeros((128, 8, 9, 128), np.float32)
    for dt in range(-4, 5):
        dr = 2 * dt + krl[:, None] - qrl[None, :]
        dc = kc[:, None] - qc[None, :]
        ok = (np.abs(dr) <= 7) & (np.abs(dc) <= 15)
        ri = np.clip(dr + 7, 0, 14)
        ci = np.clip(dc + 15, 0, 30)
        g = rpb[:, ri, ci]
        bias9[:, :, dt + 4, :] = np.where(ok[None], g, 0.0).transpose(1, 0, 2)
    masks = np.full((128, 5, 5, 128), -1e30, np.float32)
    c0 = np.clip(qc - 8, 0, 48)
    colok = (kc[:, None] >= c0[None, :]) & (kc[:, None] < c0[None, :] + 16)
    for v, j in enumerate((2, 0, 1, 30, 31)):
        kt0 = min(max(j - 2, 0), 27)
        for i in range(5):
            kr = 2 * (kt0 + i) + krl
            qr = 2 * j + qrl
            r0 = np.clip(qr - 4, 0, 56)
            rowok = (kr[:, None] >= r0[None, :]) & (kr[:, None] < r0[None, :] + 8)
            masks[:, v, i, :] = np.where(rowok & colok, 0.0, -1e30)
    return bias9.reshape(128, 8, 9 * 128), masks.reshape(128, 5, 640)


def _variant(j):
    return {0: 1, 1: 2, 30: 3, 31: 4}.get(j, 0)


NMASK = 14
M_ID, M_TRIF, M_TRIB, M_NIF, M_NIB, M_STF, M_STB, M_BD32, M_OFF, M_BLK, M_CS0, M_CS1, M_COL, M_ONES = range(14)


def _dn_masks():
    j = np.arange(128)[:, None]
    i = np.arange(128)[None, :]
    same = (j // 64) == (i // 64)
    m = np.zeros((128, NMASK, 128), np.float32)
    m[:, M_ID] = (j == i)
    m[:, M_TRIF] = same & (j <= i)
    m[:, M_TRIB] = same & (j >= i)
    m[:, M_NIF] = np.where(same & (i >= j), 0.0, -1e30)
    m[:, M_NIB] = np.where(same & (i <= j), 0.0, -1e30)
    m[:, M_STF] = same & (i > j)
    m[:, M_STB] = same & (i < j)
    m[:, M_BD32] = (j // 32) == (i // 32)
    m[:, M_OFF] = same & ((j // 32) != (i // 32))
    m[:, M_BLK] = same
    m[:, M_CS0] = (j < 64) & (i >= 0)
    m[:, M_CS1] = (j >= 64) & (i >= 0)
    m[:, M_COL, 0] = (np.arange(128) < 64)
    m[:, M_COL, 1] = (np.arange(128) >= 64)
    m[:, M_COL, 2] = 1.0
    m[:, M_ONES] = 1.0
    return m


def _dn_block(S, E, rot, bf, cm, cmb, kq, Vt, Kt, g3, ball, nball, gcall, egt, edt, egs, S32d, WTzd, QTzd,
              psG, psR, psT, psA, psB, psU, psW, oacc, sbst, d, b, h, st, upto=6):
    P2 = [128, 128]
    ident_b = cmb[:, M_ID, :]
    col = lambda t: t[:, d, b, h:h + 1]
    E("pe", lambda e: e.matmul(psG[:], lhsT=kq[:, b, 0, :], rhs=kq[:, b].rearrange("p a t -> p (a t)"), start=True, stop=True),
      ["kq"], ["psG"])
    Gsb, Gn = rot("Gsb", [128, 256], F32, 2)
    E("act", lambda e: e.activation(out=Gsb[:], in_=psG[:], func=AF.Copy), ["psG"], [Gn])
    gb, gbn = rot("gb", [128, 3, 128], BF16, 2)
    for k3 in range(3):
        E("pool", lambda e, k3=k3: e.tensor_copy(out=gb[:, k3, :], in_=col(g3[k3]).to_broadcast(P2)), ["g3"], [gbn])
    for k3 in range(3):
        E("pe", lambda e, k3=k3: e.matmul(psR[:], lhsT=gb[:, k3, :], rhs=cmb[:, M_TRIF + d, :], start=(k3 == 0), stop=(k3 == 2)),
          [gbn, "cmb"], ["psR"], inc=(k3 == 2))
    egr, egrn = rot("egr", P2, F32, 2)
    E("act", lambda e: e.activation(out=egr[:], in_=psR[:], func=AF.Exp), ["psR"], [egrn])
    if upto <= 1:
        return
    t1, t1n = rot("t1", P2, F32, 2)
    E("dve", lambda e: e.scalar_tensor_tensor(out=t1[:], in0=psR[:], scalar=col(gcall), in1=cm[:, M_NIF + d, :],
                                              op0=ALU.subtract, op1=ALU.add), ["psR", "gcall", "cm"], [t1n])
    Di, Din = rot("Di", P2, F32, 2)
    E("act", lambda e: e.activation(out=Di[:], in_=t1[:], func=AF.Exp), [t1n], [Din])
    Ds, Dsn = rot("Ds", P2, F32, 2)
    E("pool", lambda e: e.tensor_tensor(out=Ds[:], in0=Di[:], in1=cm[:, M_STF + d, :], op=ALU.mult), [Din, "cm"], [Dsn])
    N0a, N0an = rot("N0a", P2, BF16, 2)
    E("dve", lambda e: e.scalar_tensor_tensor(out=N0a[:], in0=Gsb[:, 0:128], scalar=col(nball), in1=Ds[:], op0=ALU.mult, op1=ALU.mult),
      [Gn, "nball", Dsn], [N0an])
    QKm, QKmn = rot("QKm", P2, BF16, 2)
    E("pool", lambda e: e.tensor_tensor(out=QKm[:], in0=Gsb[:, 128:256], in1=Di[:], op=ALU.mult), [Gn, Din], [QKmn])
    NX, NXn = rot("NX", [128, 256], BF16, 3)
    E("pool", lambda e, NX=NX: e.tensor_tensor(out=NX[:, 0:128], in0=N0a[:], in1=cm[:, M_BD32, :], op=ALU.mult), [N0an, "cm"], [NXn])
    E("pe", lambda e: e.transpose(out=psT[:], in_=N0a[:], identity=ident_b), [N0an, "cmb"], ["psT"])
    P0a, P0an = rot("P0a", P2, BF16, 2)
    E("act", lambda e: e.activation(out=P0a[:], in_=psT[:], func=AF.Copy), ["psT"], [P0an])
    Pm, Pmn = rot("Pm", P2, BF16, 3)
    E("pool", lambda e, Pm=Pm: e.tensor_tensor(out=Pm[:], in0=P0a[:], in1=cm[:, M_BD32, :], op=ALU.mult), [P0an, "cm"], [Pmn])
    Pof, Pofn = rot("Pof", P2, BF16, 2)
    E("pool", lambda e: e.tensor_tensor(out=Pof[:], in0=P0a[:], in1=cm[:, M_OFF, :], op=ALU.mult), [P0an, "cm"], [Pofn])
    if upto <= 2:
        return
    X, Xn = rot("X", P2, F32, 2)
    E("dve", lambda e, NX=NX: e.tensor_tensor(out=X[:], in0=NX[:, 0:128], in1=cm[:, M_ID, :], op=ALU.add), [NXn, "cm"], [Xn])
    for m in range(4):
        pa, pan = psA[m % 2], "psA%d" % (m % 2)
        if m == 0:
            E("pe", lambda e, pa=pa, Pm=Pm, NX=NX: e.matmul(pa[:, 0:128], lhsT=Pm[:], rhs=NX[:, 0:128], start=True, stop=True),
              [Pmn, NXn], [pan])
        else:
            E("pe", lambda e, pa=pa, Pm=Pm, NX=NX: e.matmul(pa[:], lhsT=Pm[:], rhs=NX[:], start=True, stop=True), [Pmn, NXn], [pan])
        E("pe", lambda e, Pm=Pm, NX=NX: e.matmul(psB[:], lhsT=NX[:, 0:128], rhs=Pm[:], start=True, stop=True), [Pmn, NXn], ["psB"])
        NX2, NX2n = rot("NX", [128, 256], BF16, 3)
        E("act", lambda e, pa=pa, NX2=NX2: e.activation(out=NX2[:, 0:128], in_=pa[:, 0:128], func=AF.Copy), [pan], [NX2n])
        if m > 0:
            E("dve", lambda e, pa=pa: e.tensor_tensor(out=X[:], in0=X[:], in1=pa[:, 128:256], op=ALU.add), [Xn, pan], [Xn])
        E("pool", lambda e, NX2=NX2: e.tensor_copy(out=NX2[:, 128:256], in_=X[:]), [Xn], [NX2n])
        Pm2, Pm2n = rot("Pm", P2, BF16, 3)
        E("dve", lambda e, Pm2=Pm2: e.tensor_copy(out=Pm2[:], in_=psB[:]), ["psB"], [Pm2n])
        NX, NXn, Pm, Pmn = NX2, NX2n, Pm2, Pm2n
    E("pe", lambda e, Pm=Pm, NX=NX: e.matmul(psA[0][:, 0:128], lhsT=Pm[:], rhs=NX[:, 128:256], start=True, stop=True), [Pmn, NXn], ["psA0"])
    E("dve", lambda e: e.tensor_tensor(out=X[:], in0=X[:], in1=psA[0][:, 0:128], op=ALU.add), [Xn, "psA0"], [Xn])
    Xb, Xbn = rot("Xb", P2, BF16, 2)
    E("pool", lambda e: e.tensor_copy(out=Xb[:], in_=X[:]), [Xn], [Xbn])
    if upto <= 3:
        return
    E("pe", lambda e: e.transpose(out=psT[:], in_=Xb[:], identity=ident_b), [Xbn, "cmb"], ["psT"])
    Tb, Tbn = rot("Tb", P2, BF16, 2)
    E("act", lambda e: e.activation(out=Tb[:], in_=psT[:], func=AF.Copy), ["psT"], [Tbn])
    E("pe", lambda e: e.matmul(psB[:], lhsT=Pof[:], rhs=Xb[:], start=True, stop=True), [Pofn, Xbn], ["psB"])
    Y1, Y1n = rot("Y1", P2, BF16, 2)
    E("act", lambda e: e.activation(out=Y1[:], in_=psB[:], func=AF.Copy), ["psB"], [Y1n])
    E("pe", lambda e: e.matmul(psB[:], lhsT=Tb[:], rhs=Y1[:], start=True, stop=True), [Tbn, Y1n], ["psB"])
    Xf, Xfn = rot("Xf", P2, BF16, 2)
    E("dve", lambda e: e.tensor_tensor(out=Xf[:], in0=X[:], in1=psB[:], op=ALU.add), [Xn, "psB"], [Xfn])
    if upto <= 4:
        return
    Kg, Kgn = rot("Kg", P2, BF16, 2)
    E("pool", lambda e: e.tensor_scalar(out=Kg[:], in0=Kt[:, b, :], scalar1=col(egt), scalar2=None, op0=ALU.mult), ["Kt", "egt"], [Kgn])
    Kd, Kdn = rot("Kd", P2, BF16, 2)
    E("pool", lambda e: e.tensor_scalar(out=Kd[:], in0=Kt[:, b, :], scalar1=col(edt), scalar2=None, op0=ALU.mult), ["Kt", "edt"], [Kdn])
    E("pe", lambda e: e.matmul(psU[:, 0:128], lhsT=Xf[:], rhs=Vt[:, b, :], start=True, stop=True), [Xfn, "Vt"], ["psU"], inc=False)
    E("pe", lambda e: e.matmul(psU[:, 128:256], lhsT=Xf[:], rhs=Kg[:], start=True, stop=True), [Xfn, Kgn], ["psU"])
    bm, bmn = rot("bm", [128, 2], F32, 2)
    E("pool", lambda e: e.tensor_tensor(out=bm[:], in0=cm[:, M_COL, 0:2], in1=col(ball).to_broadcast([128, 2]), op=ALU.mult),
      ["cm", "ball"], [bmn])
    Um = []
    for c in range(2):
        u, un = rot("Um", P2, F32, 4)
        E("dve" if c == 0 else "act",
          (lambda e, u=u, c=c: e.tensor_scalar(out=u[:], in0=psU[:, 0:128], scalar1=bm[:, c:c + 1], scalar2=None, op0=ALU.mult)) if c == 0 else
          (lambda e, u=u, c=c: e.activation(out=u[:], in_=psU[:, 0:128], func=AF.Copy, scale=bm[:, c:c + 1])),
          ["psU", bmn], [un])
        Um.append((u, un))
    Wt, Wtn = rot("Wt", P2, BF16, 2)
    E("dve", lambda e: e.tensor_scalar(out=Wt[:], in0=psU[:, 128:256], scalar1=col(ball), scalar2=None, op0=ALU.mult), ["psU", "ball"], [Wtn])
    E("pe", lambda e: e.transpose(out=psT[:], in_=Wt[:], identity=ident_b), [Wtn, "cmb"], ["psT"])
    wn = ["WTz%d%d" % (d, c) for c in range(2)]
    qn = ["QTz%d%d" % (d, c) for c in range(2)]
    E("act", lambda e: e.activation(out=WTzd[0][:, 0:64], in_=psT[:, 0:64], func=AF.Copy), ["psT"], [wn[0]])
    E("dve", lambda e: e.tensor_copy(out=WTzd[1][:, 64:128], in_=psT[:, 64:128]), ["psT"], [wn[1]])
    for c in range(2):
        cs = slice(c * 64, (c + 1) * 64)
        E("pool", lambda e, c=c, cs=cs: e.tensor_tensor(out=QTzd[c][:, cs], in0=kq[:, b, 1, cs], in1=egr[:, cs], op=ALU.mult),
          ["kq", egrn], [qn[c]])
    if upto <= 5:
        return
    if st == 0:
        sb0, sb0n = rot("Sb%d" % d, P2, BF16, 3)
        E("pool", lambda e: e.memset(sb0[:], 0.0), [], [sb0n])
        sbst[d] = (sb0, sb0n)
    Sb, Sbn = sbst[d]
    s32n = "S32_%d" % d
    hist = []
    for c in ((0, 1) if d == 0 else (1, 0)):
        E("pe", lambda e, c=c, Sb=Sb: e.matmul(psW[:, 0, :], lhsT=WTzd[c][:], rhs=Sb[:], start=True, stop=True), [wn[c], Sbn], ["psW"])
        Vn, Vnn = rot("Vn", P2, BF16, 4)
        u, un = Um[c]
        E("dve", lambda e, Vn=Vn, u=u: e.tensor_tensor(out=Vn[:], in0=u[:], in1=psW[:, 0, :], op=ALU.subtract), [un, "psW"], [Vnn])
        E("pe", lambda e, Vn=Vn: e.matmul(psW[:, 1, :], lhsT=Kd[:], rhs=Vn[:], start=True, stop=True), [Kdn, Vnn], ["psW"])
        E("dve", lambda e, c=c: e.scalar_tensor_tensor(out=S32d[:], in0=S32d[:], scalar=egs[:, d, c, b, h:h + 1], in1=psW[:, 1, :],
                                                      op0=ALU.mult, op1=ALU.add), [s32n, "egs", "psW"], [s32n])
        hist.append((c, Sb, Sbn, Vn, Vnn))
        Sb, Sbn = rot("Sb%d" % d, P2, BF16, 3)
        E("act", lambda e, Sb=Sb: e.activation(out=Sb[:], in_=S32d[:], func=AF.Copy), [s32n], [Sbn])
    sbst[d] = (Sb, Sbn)
    k = 0
    for (c, Sbc, Sbcn, Vn, Vnn) in hist:
        E("pe", lambda e, c=c, Sbc=Sbc, k=k: e.matmul(psW[:, 2, :], lhsT=QTzd[c][:], rhs=Sbc[:], start=(k == 0), stop=False),
          [qn[c], Sbcn], ["psW"], inc=False)
        E("pe", lambda e, Vn=Vn, k=k: e.matmul(psW[:, 2, :], lhsT=QKm[:], rhs=Vn[:], start=False, stop=(k == 1)),
          [QKmn, Vnn], ["psW"], inc=(k == 1))
        k += 1
    E("dve", lambda e: e.tensor_tensor(out=oacc[:, b, :], in0=oacc[:, b, :], in1=psW[:, 2, :], op=ALU.add), ["oacc", "psW"], ["oacc"])


def _stage_c(nc, S, nseq, Bscr, scr, cin, dn_steps=32, dn_heads=4, debug=False, c_upto="T", blk_upto=6):
    lvl = "PGVRT".index(c_upto)
    dqkvT, zz, ab, mix = scr["dqkvT"], scr["zz"], scr["ab"], scr["mix"]
    dbg_o = nc.dram_tensor("dbg_o", [128, 32, 128], F32, kind="ExternalOutput").ap() if debug else None
    Bdbg = Buf("dbg_o")
    with ExitStack() as es:
        B = {}

        def bf(n):
            if n not in B:
                B[n] = Buf(n)
            return B[n]

        def sb(name, shape, dt):
            return es.enter_context(nc.sbuf_tensor("c_" + name, shape, dt))

        def ps(name, shape, dt):
            return es.enter_context(nc.psum_tensor("c_" + name, shape, dt))

        def E(eng, fn, r, w, inc=True):
            S.op(eng, fn, reads=[bf(n) for n in r], writes=[bf(n) for n in w], inc=inc)

        rots = {}

        def rot(name, shape, dt, n):
            if name not in rots:
                rots[name] = [[sb("%s%d" % (name, k), shape, dt) for k in range(n)], 0]
            lst, c = rots[name]
            rots[name][1] = c + 1
            return lst[c % n], "%s%d" % (name, c % n)

        cm = sb("cm", [128, NMASK, 128], F32)
        cmb = sb("cmb", [128, NMASK, 128], BF16)
        cw = sb("cw", [128, 12, 5], F32)
        dtb = sb("dtb", [128, 8], F32)
        alog = sb("alog", [128, 8], F32)
        negA = sb("negA", [128, 8], F32)
        gD = sb("gD", [128, 128], F32)
        epsc = sb("eps", [128, 1], F32)
        onec = sb("one", [128, 1], F32)
        raw = sb("raw", [128, T + 4], F32)
        acc = sb("acc", [128, T], F32)
        kq = sb("kq", [128, 32, 2, 128], BF16)
        vTb = sb("vTb", [128, T], BF16)
        Vt = sb("Vt", [128, 32, 128], BF16)
        Kt = sb("Kt", [128, 32, 128], BF16)
        abt = sb("abt", [128, 32, 16], F32)
        tmp8 = sb("tmp8", [128, 32, 8], F32)
        gall = sb("gall", [128, 2, 32, 4], F32)
        ball = sb("ball", [128, 2, 32, 4], F32)
        nball = sb("nball", [128, 2, 32, 4], F32)
        gcall = sb("gcall", [128, 2, 32, 4], F32)
        g3 = [sb("g3_%d" % k3, [128, 2, 32, 4], BF16) for k3 in range(3)]
        gres = sb("gres", [128, 2, 32, 4], F32)
        gso = sb("gso", [128, 2, 32, 4], F32)
        egt = sb("egt", [128, 2, 32, 4], F32)
        edt = sb("edt", [128, 2, 32, 4], F32)
        egs = sb("egs", [128, 2, 2, 32, 4], F32)
        oacc = sb("oacc", [128, 32, 128], F32)
        zt = sb("zt", [128, 32, 128], F32)
        ssq = sb("ssq", [128, 32], F32)
        S32 = [sb("S32_%d" % d, [128, 128], F32) for d in range(2)]
        WTz = [[sb("WTz%d%d" % (d, c), [128, 128], BF16) for c in range(2)] for d in range(2)]
        QTz = [[sb("QTz%d%d" % (d, c), [128, 128], BF16) for c in range(2)] for d in range(2)]

        psG = ps("G", [128, 512], F32)[:, 0:256]
        psR = ps("R", [128, 512], F32)[:, 0:128]
        psT = ps("T", [128, 1024], BF16)[:, 0:128]
        psA = [ps("A%d" % k, [128, 512], F32)[:, 0:256] for k in range(2)]
        psB = ps("B", [128, 512], F32)[:, 0:128]
        psU = ps("U", [128, 512], F32)[:, 0:256]
        psW = ps("W", [128, 4, 128], F32)

        ident_b = cmb[:, M_ID, :]
        S.dma("sp", lambda e: e.dma_start(out=cm[:], in_=cin["cm"]), writes=[bf("cm")])
        S.dma("pool", lambda e: e.dma_start(out=cmb[:], in_=cin["cm"]), writes=[bf("cmb")])
        S.dma("sp", lambda e: e.dma_start(out=cw[:], in_=cin["cw"]), writes=[bf("cw")])
        S.dma("sp", lambda e: e.dma_start(out=dtb[:], in_=cin["dtb"]), writes=[bf("dtb")])
        S.dma("sp", lambda e: e.dma_start(out=alog[:], in_=cin["alog"]), writes=[bf("alog")])
        S.dma("sp", lambda e: e.dma_start(out=gD[:], in_=cin["gDb"]), writes=[bf("gD")])
        E("pool", lambda e: e.memset(epsc[:], EPS), [], ["eps"])
        E("pool", lambda e: e.memset(onec[:], 1.0), [], ["one"])
        E("pool", lambda e: e.memset(raw[:, 0:2], 0.0), [], ["raw"])
        E("pool", lambda e: e.memset(raw[:, T + 2:T + 4], 0.0), [], ["raw"])
        for d in range(2):
            for c in range(2):
                E("pool", lambda e, d=d, c=c: e.memset(WTz[d][c][:], 0.0), [], ["WTz%d%d" % (d, c)])
                E("pool", lambda e, d=d, c=c: e.memset(QTz[d][c][:], 0.0), [], ["QTz%d%d" % (d, c)])
        E("act", lambda e: e.activation(out=negA[:], in_=alog[:], func=AF.Exp), ["alog"], ["negA"])
        E("dve", lambda e: e.tensor_scalar(out=negA[:], in0=negA[:], scalar1=-1.0, scalar2=None, op0=ALU.mult), ["negA"], ["negA"])

        for s in range(nseq if lvl >= 1 else 0):
            for pc8 in range(8):
                S.dma("sp", lambda e, s=s, pc8=pc8: e.dma_start(
                    out=abt[:, pc8 * 4:(pc8 + 1) * 4, :],
                    in_=ab[s, pc8 * 512:(pc8 + 1) * 512, :].rearrange("(b p) c -> p b c", p=128)),
                    reads=[Bscr["ab"]], writes=[bf("abt")])
            E("dve", lambda e: e.tensor_tensor(out=tmp8[:], in0=abt[:, :, 0:8], in1=dtb[:].unsqueeze(1).to_broadcast([128, 32, 8]),
                                               op=ALU.add), ["abt", "dtb"], ["tmp8"])
            E("act", lambda e: e.activation(out=tmp8[:], in_=tmp8[:], func=AF.Exp), ["tmp8"], ["tmp8"])
            E("act", lambda e: e.activation(out=tmp8[:], in_=tmp8[:], func=AF.Ln, bias=onec[:], scale=1.0), ["tmp8", "one"], ["tmp8"])
            E("dve", lambda e: e.tensor_tensor(out=gall[:].rearrange("p d b h -> p b d h"),
                                               in0=tmp8[:].rearrange("p b (d h) -> p b d h", d=2),
                                               in1=negA[:].rearrange("p (d h) -> p d h", d=2).unsqueeze(1).to_broadcast([128, 32, 2, 4]),
                                               op=ALU.mult), ["tmp8", "negA"], ["gall"])
            E("act", lambda e: e.activation(out=tmp8[:], in_=abt[:, :, 8:16], func=AF.Exp, scale=-1.0), ["abt"], ["tmp8"])
            E("dve", lambda e: e.tensor_scalar(out=tmp8[:], in0=tmp8[:], scalar1=1.0, scalar2=None, op0=ALU.add), ["tmp8"], ["tmp8"])
            E("dve", lambda e: e.reciprocal(out=ball[:].rearrange("p d b h -> p b d h"),
                                            in_=tmp8[:].rearrange("p b (d h) -> p b d h", d=2)), ["tmp8"], ["ball"])
            E("dve", lambda e: e.tensor_scalar(out=nball[:], in0=ball[:], scalar1=-1.0, scalar2=None, op0=ALU.mult), ["ball"], ["nball"])
            E("dve", lambda e: e.tensor_copy(out=g3[0][:], in_=gall[:]), ["gall"], ["g3"])
            E("dve", lambda e: e.tensor_tensor(out=gres[:], in0=gall[:], in1=g3[0][:], op=ALU.subtract), ["gall", "g3"], ["gres"])
            E("dve", lambda e: e.tensor_copy(out=g3[1][:], in_=gres[:]), ["gres"], ["g3"])
            E("dve", lambda e: e.tensor_tensor(out=gres[:], in0=gres[:], in1=g3[1][:], op=ALU.subtract), ["gres", "g3"], ["gres"])
            E("dve", lambda e: e.tensor_copy(out=g3[2][:], in_=gres[:]), ["gres"], ["g3"])

            def mm3(mask_idx, d):
                for k3 in range(3):
                    E("pe", lambda e, k3=k3: e.matmul(psR[:], lhsT=cmb[:, mask_idx, :], rhs=g3[k3][:, d].rearrange("p b h -> p (b h)"),
                                                      start=(k3 == 0), stop=(k3 == 2)), ["g3", "cmb"], ["psR"], inc=(k3 == 2))
            for d in range(2):
                mm3(M_TRIF + d, d)
                E("act", lambda e, d=d: e.activation(out=gcall[:, d].rearrange("p b h -> p (b h)"), in_=psR[:], func=AF.Copy), ["psR"], ["gcall"])
                mm3(M_BLK, d)
                E("act", lambda e, d=d: e.activation(out=gso[:, d].rearrange("p b h -> p (b h)"), in_=psR[:], func=AF.Copy), ["psR"], ["gso"])
                for c in range(2):
                    mm3(M_CS0 + c, d)
                    E("act", lambda e, d=d, c=c: e.activation(out=egs[:, d, c].rearrange("p b h -> p (b h)"), in_=psR[:], func=AF.Exp),
                      ["psR"], ["egs"])
            E("act", lambda e: e.activation(out=egt[:], in_=gcall[:], func=AF.Exp), ["gcall"], ["egt"])
            E("dve", lambda e: e.tensor_tensor(out=edt[:], in0=gso[:], in1=gcall[:], op=ALU.subtract), ["gso", "gcall"], ["edt"])
            E("act", lambda e: e.activation(out=edt[:], in_=edt[:], func=AF.Exp), ["edt"], ["edt"])

            for h in range(dn_heads if lvl >= 2 else 0):
                for qi, chunk in enumerate((h, 4 + h, 8 + h)):
                    S.dma("sp", lambda e, s=s, chunk=chunk: e.dma_start(out=raw[:, 2:T + 2], in_=dqkvT[s, chunk]),
                          reads=[Bscr["dqkvT"]], writes=[bf("raw")])
                    for half in range(2):
                        eng = "dve"
                        c0 = half * 2048
                        E(eng, lambda e, c0=c0, chunk=chunk: e.tensor_scalar(
                            out=acc[:, c0:c0 + 2048], in0=raw[:, c0:c0 + 2048], scalar1=cw[:, chunk, 0:1], scalar2=None, op0=ALU.mult),
                          ["raw", "cw"], ["acc%d" % half])
                        for tap in range(1, 5):
                            E(eng, lambda e, c0=c0, chunk=chunk, tap=tap: e.scalar_tensor_tensor(
                                out=acc[:, c0:c0 + 2048], in0=raw[:, c0 + tap:c0 + tap + 2048], scalar=cw[:, chunk, tap:tap + 1],
                                in1=acc[:, c0:c0 + 2048], op0=ALU.mult, op1=ALU.add), ["raw", "cw", "acc%d" % half], ["acc%d" % half])
                    if qi == 2:
                        E("act", lambda e: e.activation(out=vTb[:], in_=acc[:], func=AF.Silu), ["acc0", "acc1"], ["vTb"])
                        for b in range(32):
                            E("pe", lambda e, b=b: e.transpose(out=psT[:], in_=vTb[:, b * 128:(b + 1) * 128], identity=ident_b),
                              ["vTb", "cmb"], ["psT"])
                            E("dve" if b % 2 else "act",
                              (lambda e, b=b: e.tensor_copy(out=Vt[:, b, :], in_=psT[:])) if b % 2 else
                              (lambda e, b=b: e.activation(out=Vt[:, b, :], in_=psT[:], func=AF.Copy)), ["psT"], ["Vt"])
                    else:
                        E("act", lambda e: e.activation(out=acc[:], in_=acc[:], func=AF.Silu), ["acc0", "acc1"], ["acc0", "acc1"])
                        for pc in range(8):
                            sqt, sqn = rot("sq", [128, 512], BF16, 2)
                            rrt, rrn = rot("rr", [128, 512], F32, 2)
                            cs = slice(pc * 512, (pc + 1) * 512)
                            E("act", lambda e, sqt=sqt, cs=cs: e.activation(out=sqt[:], in_=acc[:, cs], func=AF.Square), ["acc0", "acc1"], [sqn])
                            for hf in range(2):
                                E("pe", lambda e, sqt=sqt, hf=hf: e.matmul(psA[hf][:], lhsT=cmb[:, M_ONES, :],
                                                                         rhs=sqt[:, hf * 256:(hf + 1) * 256], start=True, stop=True),
                                  [sqn, "cmb"], ["psA%d" % hf])
                                E("act", lambda e, rrt=rrt, hf=hf: e.activation(out=rrt[:, hf * 256:(hf + 1) * 256], in_=psA[hf][:],
                                                                                func=AF.Sqrt, bias=epsc[:], scale=1.0),
                                  ["psA%d" % hf, "eps"], [rrn])
                            E("dve", lambda e, rrt=rrt: e.reciprocal(out=rrt[:], in_=rrt[:]), [rrn], [rrn])
                            sc = (128.0 ** -0.5) if qi == 0 else 1.0
                            slot = 1 if qi == 0 else 0
                            E("dve", lambda e, rrt=rrt, cs=cs, pc=pc, sc=sc, slot=slot: e.scalar_tensor_tensor(
                                out=kq[:, pc * 4:(pc + 1) * 4, slot, :], in0=acc[:, cs].rearrange("p (b t) -> p b t", t=128), scalar=sc,
                                in1=rrt[:].rearrange("p (b t) -> p b t", t=128), op0=ALU.mult, op1=ALU.mult),
                              ["acc0", "acc1", rrn], ["kq"])
                for b in range(32):
                    E("pe", lambda e, b=b: e.transpose(out=psT[:], in_=kq[:, b, 0, :], identity=ident_b), ["kq", "cmb"], ["psT"])
                    E("dve" if b % 2 else "act",
                      (lambda e, b=b: e.tensor_copy(out=Kt[:, b, :], in_=psT[:])) if b % 2 else
                      (lambda e, b=b: e.activation(out=Kt[:, b, :], in_=psT[:], func=AF.Copy)), ["psT"], ["Kt"])
                for d in range(2):
                    E("pool", lambda e, d=d: e.memset(S32[d][:], 0.0), [], ["S32_%d" % d])
                E("pool", lambda e: e.memset(oacc[:], 0.0), [], ["oacc"])
                sbst = {}
                for st in range(dn_steps if lvl >= 3 else 0):
                    for d in range(2):
                        b = st if d == 0 else 31 - st
                        _dn_block(S, E, rot, bf, cm, cmb, kq, Vt, Kt, g3, ball, nball, gcall, egt, edt, egs, S32[d], WTz[d], QTz[d],
                                  psG, psR, psT, psA, psB, psU, psW, oacc, sbst, d, b, h, st, blk_upto)
                if debug and s == 0 and h == 0:
                    for pc8 in range(8):
                        S.dma("sp", lambda e, pc8=pc8: e.dma_start(out=dbg_o[:, pc8 * 4:(pc8 + 1) * 4, :], in_=oacc[:, pc8 * 4:(pc8 + 1) * 4, :]),
                              reads=[bf("oacc")], writes=[Bdbg], sem_buf=bf("oacc"))
                if lvl < 4:
                    continue
                for pc8 in range(8):
                    S.dma("sp", lambda e, s=s, h=h, pc8=pc8: e.dma_start(
                        out=zt[:, pc8 * 4:(pc8 + 1) * 4, :],
                        in_=zz[s, pc8 * 512:(pc8 + 1) * 512, h * 128:(h + 1) * 128].rearrange("(b p) c -> p b c", p=128)),
                        reads=[Bscr["zz"]], writes=[bf("zt")])
                E("act", lambda e: e.activation(out=zt[:], in_=zt[:], func=AF.Silu), ["zt"], ["zt"])
                tq, tqn = rot("big", [128, 32, 128], F32, 1)
                E("pool", lambda e, tq=tq: e.tensor_tensor(out=tq[:], in0=oacc[:], in1=oacc[:], op=ALU.mult), ["oacc"], [tqn])
                E("dve", lambda e, tq=tq: e.tensor_reduce(out=ssq[:], in_=tq[:], axis=AX.X, op=ALU.add), [tqn], ["ssq"])
                E("act", lambda e: e.activation(out=ssq[:], in_=ssq[:], func=AF.Sqrt, bias=epsc[:], scale=1.0 / 128), ["ssq", "eps"], ["ssq"])
                E("dve", lambda e: e.reciprocal(out=ssq[:], in_=ssq[:]), ["ssq"], ["ssq"])
                E("dve", lambda e: e.tensor_tensor(out=oacc[:], in0=oacc[:], in1=ssq[:].unsqueeze(2).to_broadcast([128, 32, 128]), op=ALU.mult),
                  ["oacc", "ssq"], ["oacc"])
                E("pool", lambda e: e.tensor_tensor(out=oacc[:], in0=oacc[:], in1=gD[:].unsqueeze(1).to_broadcast([128, 32, 128]), op=ALU.mult),
                  ["oacc", "gD"], ["oacc"])
                E("dve", lambda e, tq=tq: e.tensor_tensor(out=tq[:], in0=oacc[:], in1=zt[:], op=ALU.mult), ["oacc", "zt"], [tqn])
                for pc8 in range(8):
                    r0 = s * T + pc8 * 512
                    S.dma("sp", lambda e, tq=tq, r0=r0, h=h, pc8=pc8: e.dma_start(
                        out=mix[r0:r0 + 512, 512 + h * 128:512 + (h + 1) * 128].rearrange("(b p) c -> p b c", p=128),
                        in_=tq[:, pc8 * 4:(pc8 + 1) * 4, :]),
                        reads=[bf(tqn)], writes=[Bscr["mix"]], sem_buf=bf(tqn))
        S.wait_all("sp", [Bscr["mix"], Bdbg])
        S.barrier()
        S.emit()

def _stage_d(nc, S, nseq, Bscr, x, mix, din_d, scr_d, ntiles=None):
    NT = nseq * T
    ntiles = NT // 128 if ntiles is None else ntiles
    with ExitStack() as es:
        B = {}

        def bf(n):
            if n not in B:
                B[n] = Buf(n)
            return B[n]

        def sb(name, shape, dt):
            return es.enter_context(nc.sbuf_tensor("d_" + name, shape, dt))

        def ps(name, shape, dt):
            return es.enter_context(nc.psum_tensor("d_" + name, shape, dt))

        def E(eng, fn, r, w, inc=True):
            S.op(eng, fn, reads=[bf(n) for n in r], writes=[bf(n) for n in w], inc=inc)
        wo = sb("wo", [128, 8, D], BF16)
        wr = sb("wr", [128, 8, 72], F32)
        wrh = sb("wrh", [128, 8, 72], BF16)
        wrl = sb("wrl", [128, 8, 72], BF16)
        wres = sb("wres", [128, 8, 72], F32)
        g2 = sb("g2", [128, D], F32)
        rb = sb("rb", [128, 72], F32)
        idb = sb("idb", [128, 128], BF16)
        epsc = sb("eps", [128, 1], F32)
        mt = [sb("mt%d" % i, [128, D], F32) for i in range(2)]
        xt = [sb("xt%d" % i, [128, D], F32) for i in range(2)]
        mb = sb("mb", [128, D], BF16)
        mT = sb("mT", [128, 8, 128], BF16)
        x1 = [sb("x1_%d" % i, [128, D], F32) for i in range(2)]
        junk = sb("junk", [128, D], F32)
        ss = sb("ss", [128, 1], F32)
        h2 = sb("h2", [128, D], F32)
        hh = sb("hh", [128, D], BF16)
        hl = sb("hl", [128, D], BF16)
        hres = sb("hres", [128, D], F32)
        hT = [sb("hT%d" % i, [128, 2, 8, 128], BF16) for i in range(2)]
        lg = sb("lg", [128, 72], F32)
        w8 = [sb("w8_%d" % i, [128, 8], F32) for i in range(6)]
        c1 = [sb("c1_%d" % i, [128, 1], F32) for i in range(8)]
        t64 = sb("t64", [128, 8, 8], F32)
        G = [sb("G%d" % i, [128, 8, 8], F32) for i in range(2)]
        ptr = [ps("ptr%d" % i, [128, 1024], BF16) for i in range(2)]
        po = [ps("po%d" % i, [128, 512], F32) for i in range(2)]
        pl = ps("pl", [128, 512], F32)

        for kc in range(8):
            S.dma("pool", lambda e, kc=kc: e.dma_start(out=wo[:, kc, :], in_=din_d["w_out"][kc * 128:(kc + 1) * 128, :]), writes=[bf("wo")])
        S.dma("sp", lambda e: e.dma_start(out=wr[:], in_=din_d["wr"].rearrange("(k p) c -> p k c", p=128)), writes=[bf("wr")])
        S.dma("sp", lambda e: e.dma_start(out=g2[:], in_=din_d["g2b"]), writes=[bf("g2")])
        S.dma("sp", lambda e: e.dma_start(out=rb[:], in_=din_d["rbb"]), writes=[bf("rb")])
        S.dma("pool", lambda e: e.dma_start(out=idb[:], in_=din_d["ident"]), writes=[bf("idb")])
        E("pool", lambda e: e.memset(epsc[:], EPS), [], ["eps"])
        E("dve", lambda e: e.tensor_copy(out=wrh[:], in_=wr[:]), ["wr"], ["wrh"])
        E("dve", lambda e: e.tensor_tensor(out=wres[:], in0=wr[:], in1=wrh[:], op=ALU.subtract), ["wr", "wrh"], ["wres"])
        E("dve", lambda e: e.tensor_copy(out=wrl[:], in_=wres[:]), ["wres"], ["wrl"])

        for ti in range(ntiles):
            r0 = ti * 128
            i2 = ti % 2
            MT, XT, X1, HT, GG = mt[i2], xt[i2], x1[i2], hT[i2], G[i2]
            S.dma("sp", lambda e, MT=MT, r0=r0: e.dma_start(out=MT[:], in_=mix[r0:r0 + 128, :]), reads=[Bscr["mix"]], writes=[bf("mt%d" % i2)])
            S.dma("sp", lambda e, XT=XT, r0=r0: e.dma_start(out=XT[:], in_=x[r0:r0 + 128, :]), writes=[bf("xt%d" % i2)])
            E("dve", lambda e, MT=MT: e.tensor_copy(out=mb[:], in_=MT[:]), ["mt%d" % i2], ["mb"])
            for half in range(2):
                for k4 in range(4):
                    kc = half * 4 + k4
                    E("pe", lambda e, half=half, k4=k4, kc=kc: e.transpose(out=ptr[half][:, k4 * 128:(k4 + 1) * 128],
                                                                          in_=mb[:, kc * 128:(kc + 1) * 128], identity=idb[:]),
                      ["mb", "idb"], ["ptr%d" % half], inc=(k4 == 3))
                E("act" if half else "dve",
                  (lambda e, half=half: e.activation(out=mT[:, half * 4:(half + 1) * 4, :].rearrange("p k t -> p (k t)"), in_=ptr[half][:, 0:512], func=AF.Copy)) if half else
                  (lambda e, half=half: e.tensor_copy(out=mT[:, half * 4:(half + 1) * 4, :].rearrange("p k t -> p (k t)"), in_=ptr[half][:, 0:512])),
                  ["ptr%d" % half], ["mT"])
            for ch in range(2):
                for kc in range(8):
                    E("pe", lambda e, ch=ch, kc=kc: e.matmul(po[ch][:], lhsT=mT[:, kc, :], rhs=wo[:, kc, ch * 512:(ch + 1) * 512],
                                                            start=(kc == 0), stop=(kc == 7)), ["mT", "wo"], ["po%d" % ch], inc=(kc == 7))
                E("dve", lambda e, ch=ch, X1=X1, XT=XT: e.tensor_tensor(out=X1[:, ch * 512:(ch + 1) * 512], in0=XT[:, ch * 512:(ch + 1) * 512],
                                                                     in1=po[ch][:], op=ALU.add), ["xt%d" % i2, "po%d" % ch], ["x1_%d" % i2])
            S.dma("sp", lambda e, X1=X1, r0=r0: e.dma_start(out=scr_d["x1"][r0:r0 + 128, :], in_=X1[:]),
                  reads=[bf("x1_%d" % i2)], writes=[Bscr["x1"]], sem_buf=bf("x1_%d" % i2))
            E("act", lambda e, X1=X1: e.activation(out=junk[:], in_=X1[:], func=AF.Square, accum_out=ss[:]), ["x1_%d" % i2], ["junk", "ss"])
            E("act", lambda e: e.activation(out=ss[:], in_=ss[:], func=AF.Sqrt, bias=epsc[:], scale=1.0 / D), ["ss", "eps"], ["ss"])
            E("dve", lambda e: e.reciprocal(out=ss[:], in_=ss[:]), ["ss"], ["ss"])
            E("dve", lambda e, X1=X1: e.scalar_tensor_tensor(out=h2[:], in0=X1[:], scalar=ss[:], in1=g2[:], op0=ALU.mult, op1=ALU.mult),
              ["x1_%d" % i2, "ss", "g2"], ["h2"])
            E("dve", lambda e: e.tensor_copy(out=hh[:], in_=h2[:]), ["h2"], ["hh"])
            E("dve", lambda e: e.tensor_tensor(out=hres[:], in0=h2[:], in1=hh[:], op=ALU.subtract), ["h2", "hh"], ["hres"])
            E("dve", lambda e: e.tensor_copy(out=hl[:], in_=hres[:]), ["hres"], ["hl"])
            for part, src in ((0, hh), (1, hl)):
                for half in range(2):
                    for k4 in range(4):
                        kc = half * 4 + k4
                        E("pe", lambda e, half=half, k4=k4, kc=kc, src=src: e.transpose(
                            out=ptr[half][:, k4 * 128:(k4 + 1) * 128], in_=src[:, kc * 128:(kc + 1) * 128], identity=idb[:]),
                          ["hh", "hl", "idb"], ["ptr%d" % half], inc=(k4 == 3))
                    E("act" if half else "dve",
                      (lambda e, half=half, part=part, HT=HT: e.activation(out=HT[:, part, half * 4:(half + 1) * 4, :].rearrange("p k t -> p (k t)"),
                                                                           in_=ptr[half][:, 0:512], func=AF.Copy)) if half else
                      (lambda e, half=half, part=part, HT=HT: e.tensor_copy(out=HT[:, part, half * 4:(half + 1) * 4, :].rearrange("p k t -> p (k t)"),
                                                                            in_=ptr[half][:, 0:512])),
                      ["ptr%d" % half], ["hT%d" % i2])
            S.dma("sp", lambda e, HT=HT, r0=r0: e.dma_start(
                out=scr_d["h2T"][:, :, r0:r0 + 128], in_=HT[:, 0, :, :]),
                reads=[bf("hT%d" % i2)], writes=[Bscr["h2T"]], sem_buf=bf("hT%d" % i2))
            terms = [(0, wrh), (0, wrl), (1, wrh)]
            n = 0
            for part, wpart in terms:
                for kc in range(8):
                    E("pe", lambda e, part=part, wpart=wpart, kc=kc, n=n, HT=HT: e.matmul(
                        pl[:, 0:72], lhsT=HT[:, part, kc, :], rhs=wpart[:, kc, :], start=(n == 0), stop=(n == 23)),
                      ["hT%d" % i2, "wrh", "wrl"], ["pl"], inc=(n == 23))
                    n += 1
            E("dve", lambda e: e.tensor_tensor(out=lg[:], in0=pl[:, 0:72], in1=rb[:], op=ALU.add), ["pl", "rb"], ["lg"])
            gl = lg[:, 0:8]
            el = lg[:, 8:72].rearrange("p (g e) -> p g e", e=8)
            gmax, gsum, m1, m2, gp, wa = c1[0], c1[1], c1[2], c1[3], c1[4], c1[5]
            ohg, eg_, ing, oh1, msk, oh2 = w8
            E("dve", lambda e: e.tensor_reduce(out=gmax[:], in_=gl, axis=AX.X, op=ALU.max), ["lg"], ["gmax"])
            E("dve", lambda e: e.tensor_scalar(out=ohg[:], in0=gl, scalar1=gmax[:], scalar2=None, op0=ALU.is_equal), ["lg", "gmax"], ["ohg"])
            E("dve", lambda e: e.tensor_scalar(out=eg_[:], in0=gl, scalar1=gmax[:], scalar2=None, op0=ALU.subtract), ["lg", "gmax"], ["eg"])
            E("act", lambda e: e.activation(out=eg_[:], in_=eg_[:], func=AF.Exp, accum_out=gsum[:]), ["eg"], ["eg", "gsum"])
            E("dve", lambda e: e.reciprocal(out=gp[:], in_=gsum[:]), ["gsum"], ["gp"])
            E("dve", lambda e: e.tensor_tensor(out=t64[:], in0=el, in1=ohg[:].unsqueeze(2).to_broadcast([128, 8, 8]), op=ALU.mult), ["lg", "ohg"], ["t64"])
            E("dve", lambda e: e.tensor_reduce(out=ing[:], in_=t64[:].rearrange("p g e -> p e g"), axis=AX.X, op=ALU.add), ["t64"], ["ing"])
            E("dve", lambda e: e.tensor_reduce(out=m1[:], in_=ing[:], axis=AX.X, op=ALU.max), ["ing"], ["m1"])
            E("dve", lambda e: e.tensor_scalar(out=oh1[:], in0=ing[:], scalar1=m1[:], scalar2=None, op0=ALU.is_equal), ["ing", "m1"], ["oh1"])
            E("dve", lambda e: e.scalar_tensor_tensor(out=msk[:], in0=oh1[:], scalar=-1e30, in1=ing[:], op0=ALU.mult, op1=ALU.add), ["oh1", "ing"], ["msk"])
            E("dve", lambda e: e.tensor_reduce(out=m2[:], in_=msk[:], axis=AX.X, op=ALU.max), ["msk"], ["m2"])
            E("dve", lambda e: e.tensor_scalar(out=oh2[:], in0=msk[:], scalar1=m2[:], scalar2=None, op0=ALU.is_equal), ["msk", "m2"], ["oh2"])
            E("dve", lambda e: e.tensor_tensor(out=wa[:], in0=m2[:], in1=m1[:], op=ALU.subtract), ["m1", "m2"], ["wa"])
            E("act", lambda e: e.activation(out=wa[:], in_=wa[:], func=AF.Exp), ["wa"], ["wa"])
            E("dve", lambda e: e.tensor_scalar(out=wa[:], in0=wa[:], scalar1=1.0, scalar2=None, op0=ALU.add), ["wa"], ["wa"])
            E("dve", lambda e: e.reciprocal(out=wa[:], in_=wa[:]), ["wa"], ["wa"])
            ga, gb_ = c1[6], c1[7]
            E("dve", lambda e: e.tensor_tensor(out=ga[:], in0=wa[:], in1=gp[:], op=ALU.mult), ["wa", "gp"], ["ga"])
            E("dve", lambda e: e.tensor_tensor(out=gb_[:], in0=gp[:], in1=ga[:], op=ALU.subtract), ["gp", "ga"], ["gb"])
            E("dve", lambda e: e.tensor_scalar(out=oh1[:], in0=oh1[:], scalar1=ga[:], scalar2=None, op0=ALU.mult), ["oh1", "ga"], ["oh1"])
            E("dve", lambda e: e.scalar_tensor_tensor(out=oh1[:], in0=oh2[:], scalar=gb_[:], in1=oh1[:], op0=ALU.mult, op1=ALU.add),
              ["oh2", "gb", "oh1"], ["oh1"])
            E("dve", lambda e, GG=GG: e.tensor_tensor(out=GG[:], in0=ohg[:].unsqueeze(2).to_broadcast([128, 8, 8]),
                                                     in1=oh1[:].unsqueeze(1).to_broadcast([128, 8, 8]), op=ALU.mult), ["ohg", "oh1"], ["G%d" % i2])
            S.dma("sp", lambda e, GG=GG, r0=r0: e.dma_start(out=scr_d["G"][r0:r0 + 128, :], in_=GG[:].rearrange("p g e -> p (g e)")),
                  reads=[bf("G%d" % i2)], writes=[Bscr["G"]], sem_buf=bf("G%d" % i2))
        S.wait_all("sp", [Bscr["x1"], Bscr["h2T"], Bscr["G"]])
        S.barrier()
        S.emit()


def build_d_only(ntok, ntiles):
    nc = bass.Bass("TRN2", target_bir_lowering=False)
    din = lambda name, shape: nc.dram_tensor(name, shape, F32, kind="ExternalInput").ap()
    x = din("x", [ntok, D]); mix = din("mix", [ntok, D])
    din_d = dict(w_out=din("w_out", [D, D]), wr=din("wr", [D, 72]), g2b=din("g2b", [128, D]), rbb=din("rbb", [128, 72]), ident=din("ident", [128, 128]))
    scr_d = dict(x1=nc.dram_tensor("x1", [ntok, D], F32, kind="ExternalOutput").ap(),
                 h2T=nc.dram_tensor("h2T", [128, 8, ntok], BF16, kind="ExternalOutput").ap(),
                 G=nc.dram_tensor("G", [ntok, 64], F32, kind="ExternalOutput").ap())
    Bscr = {n: Buf(n) for n in ("mix", "x1", "h2T", "G")}
    with ExitStack() as ges:
        S = Sched(nc, ges)
        _stage_d(nc, S, 1, Bscr, x, mix, din_d, scr_d, ntiles=ntiles)
    return nc


def _stage_e(nc, S, Bscr, scr_d, wE, out, ntok, experts=range(64), TG=512):
    experts = list(experts)
    with ExitStack() as es:
        B = {}

        def bf(n):
            if n not in B:
                B[n] = Buf(n)
            return B[n]

        def sb(name, shape, dt):
            return es.enter_context(nc.sbuf_tensor("e_" + name, shape, dt))

        def ps(name, shape, dt):
            return es.enter_context(nc.psum_tensor("e_" + name, shape, dt))

        def E(eng, fn, r, w, inc=True):
            S.op(eng, fn, reads=[bf(n) for n in r], writes=[bf(n) for n in w], inc=inc)
        NTL = TG // 128
        hT = sb("hT", [128, 8, TG], BF16)
        Gt = sb("Gt", [128, NTL, 64], F32)
        yacc = sb("yacc", [128, NTL, D], F32)
        wg = [sb("wg%d" % i, [128, 8, 256], BF16) for i in range(2)]
        wu = [sb("wu%d" % i, [128, 8, 256], BF16) for i in range(2)]
        wd = [sb("wd%d" % i, [128, 2, D], BF16) for i in range(2)]
        sg = [sb("sg%d" % i, [128, TG], F32) for i in range(2)]
        hid = [sb("hid%d" % i, [128, 2, TG], BF16) for i in range(2)]
        pg = [ps("pg%d" % i, [128, 512], F32) for i in range(2)]
        pu = [ps("pu%d" % i, [128, 512], F32) for i in range(2)]
        py = [ps("py%d" % i, [128, 512], F32) for i in range(2)]
        for g0 in range(0, ntok, TG):
            S.dma("sp", lambda e, g0=g0: e.dma_start(out=hT[:], in_=scr_d["h2T"][:, :, g0:g0 + TG]), reads=[Bscr["h2T"]], writes=[bf("hT")])
            S.dma("sp", lambda e, g0=g0: e.dma_start(out=Gt[:], in_=scr_d["G"][g0:g0 + TG, :].rearrange("(t p) c -> p t c", p=128)),
                  reads=[Bscr["G"]], writes=[bf("Gt")])
            S.dma("sp", lambda e, g0=g0: e.dma_start(out=yacc[:], in_=scr_d["x1"][g0:g0 + TG, :].rearrange("(t p) c -> p t c", p=128)),
                  reads=[Bscr["x1"]], writes=[bf("yacc")])
            for ie, ex in enumerate(experts):
                w2 = ie % 2
                S.dma("pool", lambda e, ex=ex, w2=w2: e.dma_start(out=wg[w2][:], in_=wE["w_gate"][ex].rearrange("(k p) c -> p k c", p=128)), writes=[bf("wg%d" % w2)])
                S.dma("pool", lambda e, ex=ex, w2=w2: e.dma_start(out=wu[w2][:], in_=wE["w_up"][ex].rearrange("(k p) c -> p k c", p=128)), writes=[bf("wu%d" % w2)])
                S.dma("pool", lambda e, ex=ex, w2=w2: e.dma_start(out=wd[w2][:], in_=wE["w_down"][ex].rearrange("(k p) c -> p k c", p=128)), writes=[bf("wd%d" % w2)])
                H = hid[w2]
                for c in range(2):
                    for kc in range(8):
                        E("pe", lambda e, c=c, kc=kc, w2=w2: e.matmul(pg[c][:, 0:TG], lhsT=wg[w2][:, kc, c * 128:(c + 1) * 128], rhs=hT[:, kc, :],
                                                                      start=(kc == 0), stop=(kc == 7)), ["wg%d" % w2, "hT"], ["pg%d" % c], inc=(kc == 7))
                    for kc in range(8):
                        E("pe", lambda e, c=c, kc=kc, w2=w2: e.matmul(pu[c][:, 0:TG], lhsT=wu[w2][:, kc, c * 128:(c + 1) * 128], rhs=hT[:, kc, :],
                                                                      start=(kc == 0), stop=(kc == 7)), ["wu%d" % w2, "hT"], ["pu%d" % c], inc=(kc == 7))
                    E("act", lambda e, c=c: e.activation(out=sg[c][:], in_=pg[c][:, 0:TG], func=AF.Silu), ["pg%d" % c], ["sg%d" % c])
                    E("dve", lambda e, c=c, H=H: e.tensor_tensor(out=H[:, c, :], in0=sg[c][:], in1=pu[c][:, 0:TG], op=ALU.mult),
                      ["sg%d" % c, "pu%d" % c], ["hid%d" % w2])
                for t in range(NTL):
                    for half in range(2):
                        p2 = (t * 2 + half) % 2
                        for c in range(2):
                            E("pe", lambda e, t=t, half=half, c=c, p2=p2, H=H, w2=w2: e.matmul(
                                py[p2][:], lhsT=H[:, c, t * 128:(t + 1) * 128], rhs=wd[w2][:, c, half * 512:(half + 1) * 512],
                                start=(c == 0), stop=(c == 1)), ["hid%d" % w2, "wd%d" % w2], ["py%d" % p2], inc=(c == 1))
                        E("dve", lambda e, t=t, half=half, p2=p2, ex=ex: e.scalar_tensor_tensor(
                            out=yacc[:, t, half * 512:(half + 1) * 512], in0=py[p2][:], scalar=Gt[:, t, ex:ex + 1],
                            in1=yacc[:, t, half * 512:(half + 1) * 512], op0=ALU.mult, op1=ALU.add), ["py%d" % p2, "Gt", "yacc"], ["yacc"])
            S.dma("sp", lambda e, g0=g0: e.dma_start(out=out[g0:g0 + TG, :].rearrange("(t p) c -> p t c", p=128), in_=yacc[:]),
                  reads=[bf("yacc")], writes=[Bscr["out"]], sem_buf=bf("yacc"))
        S.wait_all("sp", [Bscr["out"]])
        S.barrier()
        S.emit()


def build_e_only(ntok, experts):
    nc = bass.Bass("TRN2", target_bir_lowering=False)
    scr_d = dict(x1=nc.dram_tensor("x1", [ntok, D], F32, kind="ExternalInput").ap(),
                 h2T=nc.dram_tensor("h2T", [128, 8, ntok], BF16, kind="ExternalInput").ap(),
                 G=nc.dram_tensor("G", [ntok, 64], F32, kind="ExternalInput").ap())
    wE = dict(w_gate=nc.dram_tensor("w_gate", [64, D, 256], F32, kind="ExternalInput").ap(),
              w_up=nc.dram_tensor("w_up", [64, D, 256], F32, kind="ExternalInput").ap(),
              w_down=nc.dram_tensor("w_down", [64, 256, D], F32, kind="ExternalInput").ap())
    out = nc.dram_tensor("out", [ntok, D], F32, kind="ExternalOutput").ap()
    Bscr = {n: Buf(n) for n in ("x1", "h2T", "G", "out")}
    with ExitStack() as ges:
        S = Sched(nc, ges)
        _stage_e(nc, S, Bscr, scr_d, wE, out, ntok, experts=experts, TG=ntok)
    return nc


def build_c_only(nseq=1, dn_steps=32, dn_heads=4, c_upto="T", blk_upto=6, debug=True):
    nc = bass.Bass("TRN2", target_bir_lowering=False)
    NT = nseq * T
    din = lambda name, shape: nc.dram_tensor(name, shape, F32, kind="ExternalInput").ap()
    scr = dict(dqkvT=din("dqkvT", [nseq, 12, 128, T]), zz=din("zz", [nseq, T, 512]), ab=din("ab", [nseq, T, 16]),
               mix=nc.dram_tensor("mix", [NT, D], F32, kind="ExternalOutput").ap())
    cin = dict(cw=din("cw", [128, 12, 5]), cm=din("cm", [128, NMASK, 128]), dtb=din("dtb", [128, 8]),
               alog=din("alog", [128, 8]), gDb=din("gDb", [128, 128]))
    Bscr = {n: Buf(n) for n in ("dqkvT", "zz", "ab", "mix")}
    with ExitStack() as ges:
        S = Sched(nc, ges)
        _stage_c(nc, S, nseq, Bscr, scr, cin, dn_steps, dn_heads, debug, c_upto, blk_upto)
    return nc


def build(nseq, debug=False, stages="ABCDE", dn_steps=32, dn_heads=4, c_upto="T", blk_upto=6, e_experts=range(64)):
    nc = bass.Bass("TRN2", target_bir_lowering=False)
    NT = nseq * T
    okind = "ExternalOutput" if debug else "Internal"

    def din(name, shape, dt=F32):
        return nc.dram_tensor(name, shape, dt, kind="ExternalInput").ap()

    x = din("x", [NT, D])
    w_in = din("w_in", [D, INC])
    g1b = din("g1b", [128, D])
    gq2 = din("gq2", [128, 1])
    gk2 = din("gk2", [128, 1])
    bones = din("bones", [128, 128])
    ident = din("ident", [128, 128])
    bias9 = din("bias9", [128, 8, 1152])
    amask = din("amask", [128, 5, 640])
    gAb = din("gAb", [128, 512])
    cin = dict(cw=din("cw", [128, 12, 5]), cm=din("cm", [128, NMASK, 128]), dtb=din("dtb", [128, 8]),
               alog=din("alog", [128, 8]), gDb=din("gDb", [128, 128]))

    qT = nc.dram_tensor("qT", [nseq, 4, 128, T], BF16, kind=okind).ap()
    kT = nc.dram_tensor("kT", [nseq, 4, 128, T], BF16, kind=okind).ap()
    vp = nc.dram_tensor("vp", [nseq, T, 520], BF16, kind=okind).ap()
    dqkvT = nc.dram_tensor("dqkvT", [nseq, 12, 128, T], F32, kind=okind).ap()
    zz = nc.dram_tensor("zz", [nseq, T, 512], F32, kind=okind).ap()
    ab = nc.dram_tensor("ab", [nseq, T, 16], F32, kind=okind).ap()
    mix = nc.dram_tensor("mix", [NT, D], F32, kind=okind).ap()
    full = ("D" in stages) and ("E" in stages)
    if full:
        din_d = dict(w_out=din("w_out", [D, D]), wr=din("wr", [D, 72]), g2b=din("g2b", [128, D]), rbb=din("rbb", [128, 72]), ident=ident)
        wE = dict(w_gate=din("w_gate", [64, D, 256]), w_up=din("w_up", [64, D, 256]), w_down=din("w_down", [64, 256, D]))
        scr_d = dict(x1=nc.dram_tensor("x1", [NT, D], F32, kind=okind).ap(),
                     h2T=nc.dram_tensor("h2T", [128, 8, NT], BF16, kind=okind).ap(),
                     G=nc.dram_tensor("G", [NT, 64], F32, kind=okind).ap())
        out = nc.dram_tensor("out", [NT, D], F32, kind="ExternalOutput").ap()

    Bscr = {n: Buf(n) for n in ("qT", "kT", "vp", "dqkvT", "zz", "ab", "mix", "x1", "h2T", "G", "out")}

    with ExitStack() as ges:
        S = Sched(nc, ges)

        with ExitStack() as es:
            def sb(name, shape, dt):
                return es.enter_context(nc.sbuf_tensor(name, shape, dt))

            def ps(name, shape, dt):
                return es.enter_context(nc.psum_tensor(name, shape, dt))
            wsb = sb("wsb", [128, 8, INC], BF16)
            g1 = sb("g1", [128, D], F32)
            gq = sb("gq", [128, 1], F32)
            gk = sb("gk", [128, 1], F32)
            gq8 = sb("gq8", [128, 1], F32)
            gk8 = sb("gk8", [128, 1], F32)
            bo = sb("bo", [128, 128], BF16)
            idb = sb("idb", [128, 128], BF16)
            epsc = sb("epsc", [128, 1], F32)
            xg = [sb("xg%d" % i, [128, 4, D], F32) for i in range(2)]
            junk = sb("junk", [128, D], F32)
            ss = sb("ss", [128, 4], F32)
            rstd = sb("rstd", [128, 4], F32)
            hb = sb("hb", [128, 4, D], BF16)
            hT = [sb("hT%d" % i, [128, 8, 512], BF16) for i in range(2)]
            sq = [sb("sq%d" % i, [128, 512], BF16) for i in range(2)]
            rr = [sb("rr%d" % i, [128, 512], F32) for i in range(2)]
            oqk = [sb("oqk%d" % i, [128, 512], BF16) for i in range(3)]
            of = [sb("of%d" % i, [128, 512], F32) for i in range(3)]
            vt = [sb("vt%d" % i, [128, 8, 65], BF16) for i in range(2)]
            oab = [sb("oab%d" % i, [128, 16], F32) for i in range(2)]
            ptr = [ps("ptr%d" % i, [128, 512], BF16) for i in range(2)]
            pf = [ps("pf%d" % i, [128, 512], F32) for i in range(3)]
            pn = [ps("pn%d" % i, [128, 512], F32) for i in range(2)]

            B = {}

            def bf(n):
                if n not in B:
                    B[n] = Buf(n)
                return B[n]

            for kc in range(8):
                S.dma("pool", lambda e, kc=kc: e.dma_start(out=wsb[:, kc, :], in_=w_in[kc * 128:(kc + 1) * 128, :]),
                      writes=[bf("w%d" % kc)])
            S.dma("sp", lambda e: e.dma_start(out=g1[:], in_=g1b), writes=[bf("g1")])
            S.dma("sp", lambda e: e.dma_start(out=gq[:], in_=gq2), writes=[bf("gq")])
            S.dma("sp", lambda e: e.dma_start(out=gk[:], in_=gk2), writes=[bf("gk")])
            S.dma("pool", lambda e: e.dma_start(out=bo[:], in_=bones), writes=[bf("bo")])
            S.dma("pool", lambda e: e.dma_start(out=idb[:], in_=ident), writes=[bf("idb")])
            S.op("pool", lambda e: e.memset(epsc[:], EPS), writes=[bf("eps")])
            S.op("dve", lambda e: e.tensor_scalar(out=gq8[:], in0=gq[:], scalar1=0.125, scalar2=None, op0=ALU.mult),
                 reads=[bf("gq")], writes=[bf("gq8")])
            S.op("dve", lambda e: e.tensor_scalar(out=gk8[:], in0=gk[:], scalar1=8.0, scalar2=None, op0=ALU.mult),
                 reads=[bf("gk")], writes=[bf("gk8")])
            for i in range(2):
                S.op("pool", lambda e, i=i: e.memset(vt[i][:, :, 64:65], 1.0), writes=[bf("vt%d" % i)])
            wall = [bf("w%d" % kc) for kc in range(8)]

            cnt = {"pf": 0, "pn": 0, "sq": 0, "oqk": 0, "of": 0, "vt": 0, "oab": 0, "ptr": 0}

            def rot(name, n):
                i = cnt[name] % n
                cnt[name] += 1
                return i

            for s in range(nseq):
                for g in range(8):
                    gi = s * 8 + g
                    tok0 = s * T + g * 512
                    X, BX = xg[gi % 2], bf("xg%d" % (gi % 2))
                    H, BH = hT[gi % 2], bf("hT%d" % (gi % 2))
                    S.dma("sp", lambda e, X=X, tok0=tok0: e.dma_start(
                        out=X[:], in_=x[tok0:tok0 + 512, :].rearrange("(t p) d -> p t d", p=128)), writes=[BX])
                    for t in range(4):
                        S.op("act", lambda e, X=X, t=t: e.activation(out=junk[:], in_=X[:, t, :], func=AF.Square,
                                                                      accum_out=ss[:, t:t + 1]),
                             reads=[BX], writes=[bf("junk"), bf("ss")])
                    S.op("act", lambda e: e.activation(out=rstd[:], in_=ss[:], func=AF.Sqrt, bias=epsc[:], scale=1.0 / D),
                         reads=[bf("ss"), bf("eps")], writes=[bf("rstd")])
                    S.op("dve", lambda e: e.reciprocal(out=rstd[:], in_=rstd[:]), reads=[bf("rstd")], writes=[bf("rstd")])
                    for t in range(4):
                        S.op("dve", lambda e, X=X, t=t: e.scalar_tensor_tensor(
                            out=hb[:, t, :], in0=X[:, t, :], scalar=rstd[:, t:t + 1], in1=g1[:], op0=ALU.mult, op1=ALU.mult),
                            reads=[BX, bf("rstd"), bf("g1")], writes=[bf("hb")])
                    for kc in range(8):
                        pi = rot("ptr", 2)
                        for t in range(4):
                            S.op("pe", lambda e, pi=pi, t=t, kc=kc: e.transpose(
                                out=ptr[pi][:, t * 128:(t + 1) * 128], in_=hb[:, t, kc * 128:(kc + 1) * 128], identity=idb[:]),
                                reads=[bf("hb"), bf("idb")], writes=[bf("ptr%d" % pi)], inc=(t == 3))
                        S.op("act" if kc % 2 else "dve",
                             (lambda e, pi=pi, kc=kc, H=H: e.activation(out=H[:, kc, :], in_=ptr[pi][:], func=AF.Copy)) if kc % 2 else
                             (lambda e, pi=pi, kc=kc, H=H: e.tensor_copy(out=H[:, kc, :], in_=ptr[pi][:])),
                             reads=[bf("ptr%d" % pi)], writes=[BH])
                    for c in list(range(8)) + list(range(12, 24)):
                        pi = rot("pf", 3)
                        for kc in range(8):
                            S.op("pe", lambda e, pi=pi, kc=kc, c=c, H=H: e.matmul(
                                pf[pi][:], lhsT=wsb[:, kc, c * 128:(c + 1) * 128], rhs=H[:, kc, :], start=(kc == 0), stop=(kc == 7)),
                                reads=[BH] + (wall if kc == 0 else []), writes=[bf("pf%d" % pi)], inc=(kc == 7))
                        if c < 8:
                            si, ni, oi = rot("sq", 2), rot("pn", 2), rot("oqk", 3)
                            S.op("act", lambda e, pi=pi, si=si: e.activation(out=sq[si][:], in_=pf[pi][:], func=AF.Square),
                                 reads=[bf("pf%d" % pi)], writes=[bf("sq%d" % si)])
                            S.op("pe", lambda e, ni=ni, si=si: e.matmul(pn[ni][:], lhsT=bo[:], rhs=sq[si][:], start=True, stop=True),
                                 reads=[bf("sq%d" % si), bf("bo")], writes=[bf("pn%d" % ni)])
                            S.op("act", lambda e, ni=ni: e.activation(out=rr[ni][:], in_=pn[ni][:], func=AF.Sqrt,
                                                                       bias=epsc[:], scale=1.0 / 64),
                                 reads=[bf("pn%d" % ni), bf("eps")], writes=[bf("rr%d" % ni)])
                            S.op("dve", lambda e, ni=ni: e.reciprocal(out=rr[ni][:], in_=rr[ni][:]),
                                 reads=[bf("rr%d" % ni)], writes=[bf("rr%d" % ni)])
                            gcol = gq8 if c < 4 else gk
                            S.op("dve", lambda e, pi=pi, ni=ni, oi=oi, gcol=gcol: e.scalar_tensor_tensor(
                                out=oqk[oi][:], in0=pf[pi][:], scalar=gcol[:], in1=rr[ni][:], op0=ALU.mult, op1=ALU.mult),
                                reads=[bf("pf%d" % pi), bf("rr%d" % ni), bf("gq8"), bf("gk")], writes=[bf("oqk%d" % oi)])
                            dst = (qT if c < 4 else kT)[s, c % 4, :, g * 512:(g + 1) * 512]
                            S.dma("sp", lambda e, oi=oi, dst=dst: e.dma_start(out=dst, in_=oqk[oi][:]),
                                  reads=[bf("oqk%d" % oi)], writes=[Bscr["qT" if c < 4 else "kT"]], sem_buf=bf("oqk%d" % oi))
                        else:
                            oi = rot("of", 3)
                            S.op("act", lambda e, pi=pi, oi=oi: e.activation(out=of[oi][:], in_=pf[pi][:], func=AF.Copy),
                                 reads=[bf("pf%d" % pi)], writes=[bf("of%d" % oi)])
                            dst = dqkvT[s, c - 12, :, g * 512:(g + 1) * 512]
                            S.dma("sp", lambda e, oi=oi, dst=dst: e.dma_start(out=dst, in_=of[oi][:]),
                                  reads=[bf("of%d" % oi)], writes=[Bscr["dqkvT"]], sem_buf=bf("of%d" % oi))
                    for t in range(4):
                        r0 = g * 512 + t * 128
                        for which, c0, ncol in (("v", 1024, 512), ("z", 3072, 512), ("ab", 3584, 16)):
                            pi = rot("pf", 3)
                            for kc in range(8):
                                S.op("pe", lambda e, pi=pi, kc=kc, t=t, c0=c0, ncol=ncol, H=H: e.matmul(
                                    pf[pi][:, 0:ncol], lhsT=H[:, kc, t * 128:(t + 1) * 128], rhs=wsb[:, kc, c0:c0 + ncol],
                                    start=(kc == 0), stop=(kc == 7)),
                                    reads=[BH], writes=[bf("pf%d" % pi)], inc=(kc == 7))
                            if which == "v":
                                oi = rot("vt", 2)
                                S.op("dve", lambda e, pi=pi, oi=oi: e.tensor_copy(
                                    out=vt[oi][:, :, 0:64], in_=pf[pi][:].rearrange("p (h d) -> p h d", d=64)),
                                    reads=[bf("pf%d" % pi)], writes=[bf("vt%d" % oi)])
                                S.dma("sp", lambda e, oi=oi, r0=r0, s=s: e.dma_start(
                                    out=vp[s, r0:r0 + 128, :], in_=vt[oi][:].rearrange("p h e -> p (h e)")),
                                    reads=[bf("vt%d" % oi)], writes=[Bscr["vp"]], sem_buf=bf("vt%d" % oi))
                            elif which == "z":
                                oi = rot("of", 3)
                                S.op("act", lambda e, pi=pi, oi=oi: e.activation(out=of[oi][:], in_=pf[pi][:], func=AF.Copy),
                                     reads=[bf("pf%d" % pi)], writes=[bf("of%d" % oi)])
                                S.dma("sp", lambda e, oi=oi, r0=r0, s=s: e.dma_start(out=zz[s, r0:r0 + 128, :], in_=of[oi][:]),
                                      reads=[bf("of%d" % oi)], writes=[Bscr["zz"]], sem_buf=bf("of%d" % oi))
                            else:
                                oi = rot("oab", 2)
                                S.op("dve", lambda e, pi=pi, oi=oi: e.tensor_copy(out=oab[oi][:], in_=pf[pi][:, 0:16]),
                                     reads=[bf("pf%d" % pi)], writes=[bf("oab%d" % oi)])
                                S.dma("sp", lambda e, oi=oi, r0=r0, s=s: e.dma_start(out=ab[s, r0:r0 + 128, :], in_=oab[oi][:]),
                                      reads=[bf("oab%d" % oi)], writes=[Bscr["ab"]], sem_buf=bf("oab%d" % oi))
            S.wait_all("sp", list(Bscr.values()))
            S.emit()

        S.barrier()
        with ExitStack() as es:
            def sb(name, shape, dt):
                return es.enter_context(nc.sbuf_tensor(name, shape, dt))

            def ps(name, shape, dt):
                return es.enter_context(nc.psum_tensor(name, shape, dt))
            qs = sb("qs", [128, 4, T], BF16)
            ks = sb("ks", [128, 4, T], BF16)
            vs = sb("vs", [128, 32, 520], BF16)
            b9 = sb("b9", [128, 8, 1152], F32)
            am = sb("am", [128, 5, 640], F32)
            gA = sb("gA", [128, 512], F32)
            epsc = sb("epsc2", [128, 1], F32)
            t1 = [sb("t1_%d" % i, [128, 640], F32) for i in range(2)]
            t2 = [sb("t2_%d" % i, [128, 640], F32) for i in range(2)]
            pT = [sb("pT%d" % i, [128, 640], BF16) for i in range(2)]
            rden = sb("rden", [128, 2, 4], F32)
            oo = sb("oo", [128, 512], F32)
            junk = sb("junk2", [128, 512], F32)
            s1 = sb("s1", [128, 1], F32)
            om = [sb("om%d" % i, [128, 512], F32) for i in range(2)]
            st = [ps("st%d" % i, [128, 1024], F32) for i in range(2)]
            po = [ps("po%d" % i, [128, 2, 512], F32) for i in range(2)]
            B = {}

            def bf(n):
                if n not in B:
                    B[n] = Buf(n)
                return B[n]
            S.dma("sp", lambda e: e.dma_start(out=b9[:], in_=bias9), writes=[bf("b9")])
            S.dma("sp", lambda e: e.dma_start(out=am[:], in_=amask), writes=[bf("am")])
            S.dma("sp", lambda e: e.dma_start(out=gA[:], in_=gAb), writes=[bf("gA")])
            S.op("pool", lambda e: e.memset(epsc[:], EPS), writes=[bf("eps")])
            it = 0
            for s in range(nseq):
                for c in range(4):
                    S.dma("sp", lambda e, c=c, s=s: e.dma_start(out=qs[:, c, :], in_=qT[s, c]), reads=[Bscr["qT"]], writes=[bf("qs")])
                    S.dma("sp", lambda e, c=c, s=s: e.dma_start(out=ks[:, c, :], in_=kT[s, c]), reads=[Bscr["kT"]], writes=[bf("ks")])
                for q4 in range(4):
                    S.dma("sp", lambda e, q4=q4, s=s: e.dma_start(
                        out=vs[:, q4 * 8:(q4 + 1) * 8, :],
                        in_=vp[s, q4 * 1024:(q4 + 1) * 1024, :].rearrange("(t p) e -> p t e", p=128)),
                        reads=[Bscr["vp"]], writes=[bf("vs")])
                for j in range(32):
                    kt0 = min(max(j - 2, 0), 27)
                    v = _variant(j)
                    d0 = kt0 - j + 4
                    PO, BPO = po[j % 2], bf("po%d" % (j % 2))
                    for h in range(8):
                        hp, p0 = h // 2, (h % 2) * 64
                        i2 = it % 2
                        it += 1
                        ST, BST = st[i2], bf("st%d" % i2)
                        for i in range(5):
                            S.op("pe", lambda e, ST=ST, i=i, hp=hp, p0=p0, kt0=kt0, j=j: e.matmul(
                                ST[:, i * 128:(i + 1) * 128],
                                lhsT=ks[p0:p0 + 64, hp, (kt0 + i) * 128:(kt0 + i + 1) * 128],
                                rhs=qs[p0:p0 + 64, hp, j * 128:(j + 1) * 128], start=True, stop=True),
                                reads=[bf("qs"), bf("ks")], writes=[BST], inc=(i == 4))
                        S.op("dve", lambda e, ST=ST, i2=i2, h=h, d0=d0: e.tensor_tensor(
                            out=t1[i2][:], in0=ST[:, 0:640], in1=b9[:, h, d0 * 128:d0 * 128 + 640], op=ALU.add),
                            reads=[BST, bf("b9")], writes=[bf("t1_%d" % i2)])
                        S.op("pool", lambda e, i2=i2, v=v: e.tensor_tensor(
                            out=t2[i2][:], in0=t1[i2][:], in1=am[:, v, :], op=ALU.add),
                            reads=[bf("t1_%d" % i2), bf("am")], writes=[bf("t2_%d" % i2)])
                        S.op("act", lambda e, i2=i2: e.activation(out=pT[i2][:], in_=t2[i2][:], func=AF.Exp),
                             reads=[bf("t2_%d" % i2)], writes=[bf("pT%d" % i2)])
                        for i in range(5):
                            S.op("pe", lambda e, PO=PO, i=i, h=h, i2=i2, kt0=kt0: e.matmul(
                                PO[:, h // 4, (h % 4) * 65:(h % 4) * 65 + 65],
                                lhsT=pT[i2][:, i * 128:(i + 1) * 128], rhs=vs[:, kt0 + i, h * 65:(h + 1) * 65],
                                start=(i == 0), stop=(i == 4)),
                                reads=[bf("pT%d" % i2), bf("vs")], writes=[BPO], inc=(i == 4))
                    pov = PO[:, :, 0:260].rearrange("p a (h e) -> p a h e", e=65)
                    S.op("dve", lambda e, pov=pov: e.reciprocal(out=rden[:], in_=pov[:, :, :, 64]),
                         reads=[BPO], writes=[bf("rden")])
                    S.op("dve", lambda e, pov=pov: e.tensor_tensor(
                        out=oo[:].rearrange("p (a h d) -> p a h d", a=2, h=4), in0=pov[:, :, :, 0:64],
                        in1=rden[:].unsqueeze(3).to_broadcast([128, 2, 4, 64]), op=ALU.mult),
                        reads=[BPO, bf("rden")], writes=[bf("oo")])
                    S.op("act", lambda e: e.activation(out=junk[:], in_=oo[:], func=AF.Square, accum_out=s1[:]),
                         reads=[bf("oo")], writes=[bf("junk"), bf("s1")])
                    S.op("act", lambda e: e.activation(out=s1[:], in_=s1[:], func=AF.Sqrt, bias=epsc[:], scale=1.0 / 512),
                         reads=[bf("s1"), bf("eps")], writes=[bf("s1")])
                    S.op("dve", lambda e: e.reciprocal(out=s1[:], in_=s1[:]), reads=[bf("s1")], writes=[bf("s1")])
                    OM, BOM = om[j % 2], bf("om%d" % (j % 2))
                    S.op("dve", lambda e, OM=OM: e.scalar_tensor_tensor(
                        out=OM[:], in0=oo[:], scalar=s1[:], in1=gA[:], op0=ALU.mult, op1=ALU.mult),
                        reads=[bf("oo"), bf("s1"), bf("gA")], writes=[BOM])
                    r0 = s * T + j * 128
                    S.dma("sp", lambda e, OM=OM, r0=r0: e.dma_start(out=mix[r0:r0 + 128, 0:512], in_=OM[:]),
                          reads=[BOM], writes=[Bscr["mix"]], sem_buf=BOM)
            S.wait_all("sp", [Bscr["mix"]])
            S.emit()

        if "C" in stages:
            S.barrier()
            _stage_c(nc, S, nseq, Bscr, dict(dqkvT=dqkvT, zz=zz, ab=ab, mix=mix), cin, dn_steps, dn_heads, debug, c_upto, blk_upto)
        if full:
            S.barrier()
            _stage_d(nc, S, nseq, Bscr, x, mix, din_d, scr_d)
            S.barrier()
            _stage_e(nc, S, Bscr, scr_d, wE, out, NT, experts=e_experts, TG=512)
    return nc


def _prep(inputs):
    f = lambda a: np.ascontiguousarray(np.asarray(a, dtype=np.float32))
    c = {}
    c["w_in"] = f(inputs["w_in"][0])
    c["g1b"] = f(np.broadcast_to(inputs["ln1_g"][0][None, :], (128, D)))
    c["gq2"] = f(np.tile(inputs["attn_q_norm_g"][0], 2)[:, None])
    c["gk2"] = f(np.tile(inputs["attn_k_norm_g"][0], 2)[:, None])
    p = np.arange(128)
    c["bones"] = f((p[:, None] // 64) == (p[None, :] // 64))
    c["ident"] = f(np.eye(128))
    b9, am = _attn_tables(np.asarray(inputs["attn_rpb"][0], np.float32))
    c["bias9"], c["amask"] = f(b9), f(am)
    c["gAb"] = f(np.broadcast_to(inputs["attn_out_norm_g"][0][None, :], (128, 512)))
    c["cw"] = f(np.asarray(inputs["dn_conv_w"][0]).T.reshape(12, 128, 5).transpose(1, 0, 2))
    c["cm"] = f(_dn_masks())
    c["dtb"] = f(np.broadcast_to(np.asarray(inputs["dn_dt_bias"][0]).reshape(1, 8), (128, 8)))
    c["alog"] = f(np.broadcast_to(np.asarray(inputs["dn_a_log"][0]).reshape(1, 8), (128, 8)))
    c["gDb"] = f(np.broadcast_to(np.asarray(inputs["dn_out_norm_g"][0])[None, :], (128, 128)))
    return c


def _prep_d(inputs):
    f = lambda a: np.ascontiguousarray(np.asarray(a, dtype=np.float32))
    c = {}
    c["w_out"] = f(inputs["w_out"][0])
    c["wr"] = f(np.concatenate([inputs["router_group_w"][0], inputs["router_expert_w"][0]], axis=1))
    c["g2b"] = f(np.broadcast_to(inputs["ln2_g"][0][None, :], (128, D)))
    c["rbb"] = f(np.broadcast_to(np.concatenate([inputs["router_group_b"][0], inputs["router_expert_b"][0]])[None, :], (128, 72)))
    c["ident"] = f(np.eye(128))
    return c


def _in_maps(inputs, n=8, nseq=2):
    x = np.asarray(inputs["x"], np.float32).reshape(16 * T, D)
    consts = dict(_prep(inputs))
    consts.update(_prep_d(inputs))
    f = lambda a: np.ascontiguousarray(np.asarray(a, dtype=np.float32))
    consts["w_gate"] = f(inputs["expert_w_gate"][0])
    consts["w_up"] = f(inputs["expert_w_up"][0])
    consts["w_down"] = f(inputs["expert_w_down"][0])
    maps = []
    for i in range(n):
        m = dict(consts)
        m["x"] = np.ascontiguousarray(x[i * nseq * T:(i + 1) * nseq * T])
        maps.append(m)
    return maps


def kernel(**inputs):
    nc = build(2)
    res = run_bass_kernel_spmd(nc, _in_maps(inputs), core_ids=list(range(8)))
    out = np.concatenate([np.asarray(r["out"]) for r in res.results], axis=0)
    return np.ascontiguousarray(out.reshape(16, T, D).astype(np.float32))
```

```python
from contextlib import ExitStack
import numpy as np
import concourse.bass as bass
import concourse.mybir as mybir
from concourse.bass_utils import run_bass_kernel_spmd

F32 = mybir.dt.float32
BF16 = mybir.dt.bfloat16
AF = mybir.ActivationFunctionType
ALU = mybir.AluOpType
AX = mybir.AxisListType

D = 1024
T = 4096
INC = 3600
EPS = 1e-6
ENGS = ("pe", "act", "dve", "pool", "sp")


class Buf:
    __slots__ = ("name", "last_w", "reads", "sem")

    def __init__(self, name):
        self.name = name
        self.last_w = None
        self.reads = []
        self.sem = None


class Sched:
    def __init__(self, nc, es, n_hw_sems=64, n_sw_sems=16):
        self.nc = nc
        self.sems = {e: es.enter_context(nc.semaphore("s_" + e)) for e in ENGS}
        for i in range(n_hw_sems):
            self.sems[("hw", i)] = es.enter_context(nc.semaphore("s_hw%d" % i))
        for i in range(n_sw_sems):
            self.sems[("sw", i)] = es.enter_context(nc.semaphore("s_sw%d" % i))
        self.n_sems = {"hw": n_hw_sems, "sw": n_sw_sems}
        self._nd = {"hw": 0, "sw": 0}
        self.ops = {e: [] for e in ENGS}
        self.tick = {e: 0 for e in ENGS}
        self.known = {e: {} for e in ENGS}
        self.dma_cnt = {}

    def _deps(self, eng, reads, writes):
        deps = {}

        def add(ev):
            if ev is None:
                return
            k, c = ev
            if k == "pe" and eng == "pe":
                return
            if deps.get(k, 0) < c:
                deps[k] = c
        for b in reads:
            add(b.last_w)
            if b.name[:2] in ("ps", "pf", "pn", "po", "st", "pt", "pl", "pg", "pu", "py"):
                for r in b.reads:
                    if r[0] != eng:
                        add(r)
        for b in writes:
            add(b.last_w)
            for r in b.reads:
                add(r)
        kn = self.known[eng]
        out = []
        for k, c in deps.items():
            if kn.get(k, 0) >= c:
                continue
            kn[k] = c
            out.append((k, c))
        return out

    def _commit(self, ev, reads, writes):
        for b in reads:
            b.reads.append(ev)
            if len(b.reads) > 32:
                m = {}
                for k, c in b.reads:
                    if m.get(k, 0) < c:
                        m[k] = c
                b.reads = list(m.items())
        for b in writes:
            b.last_w = ev
            b.reads = []

    def op(self, eng, fn, reads=(), writes=(), inc=True):
        waits = self._deps(eng, reads, writes)
        if inc:
            self.tick[eng] += 1
            ev = (eng, self.tick[eng])
            self.ops[eng].append((waits, fn, (eng, 1)))
        else:
            ev = (eng, self.tick[eng] + 1)
            self.ops[eng].append((waits, fn, None))
        self._commit(ev, reads, writes)
        return ev

    def dma(self, eng, fn, reads=(), writes=(), sem_buf=None):
        if sem_buf is None:
            sem_buf = (list(writes) + list(reads))[0]
        cls = "sw" if eng == "pool" else "hw"
        if sem_buf.sem is None:
            sem_buf.sem = (cls, self._nd[cls] % self.n_sems[cls])
            self._nd[cls] += 1
        k = sem_buf.sem
        assert k[0] == cls, "buffer %s mixes software- and hardware-DGE DMAs on one semaphore" % sem_buf.name
        waits = self._deps(eng, reads, writes)
        prev = self.dma_cnt.get(k, 0)
        if prev and self.known[eng].get(k, 0) < prev:
            self.known[eng][k] = prev
            waits.append((k, prev))
        cnt = prev + 16
        self.dma_cnt[k] = cnt
        ev = (k, cnt)
        self.ops[eng].append((waits, fn, (k, 16)))
        self._commit(ev, reads, writes)
        return ev

    def barrier(self):
        for eng in ENGS:
            waits = []
            kn = self.known[eng]
            for e2 in ENGS:
                c = self.tick[e2]
                if e2 != "sp" and c and kn.get(e2, 0) < c and not (e2 == "pe" and eng == "pe"):
                    kn[e2] = c
                    waits.append((e2, c))
            for k, c in self.dma_cnt.items():
                if kn.get(k, 0) < c:
                    kn[k] = c
                    waits.append((k, c))
            if waits:
                self.ops[eng].append((waits, None, None))

    def wait_all(self, eng, bufs):
        self.ops[eng].append((self._deps(eng, bufs, bufs), None, None))

    def emit(self):
        nc, sems, ops = self.nc, self.sems, self.ops

        def run(engine, lst):
            for waits, fn, inc in lst:
                for k, c in waits:
                    engine.wait_ge(sems[k], c)
                if fn is None:
                    continue
                inst = fn(engine)
                if inc is not None:
                    inst.then_inc(sems[inc[0]], inc[1])

        with nc.Block() as block:
            @block.tensor
            def _(e):
                run(e, ops["pe"])

            @block.scalar
            def _(e):
                run(e, ops["act"])

            @block.vector
            def _(e):
                run(e, ops["dve"])

            @block.gpsimd
            def _(e):
                run(e, ops["pool"])

            @block.sync
            def _(e):
                run(e, ops["sp"])
        self.ops = {e: [] for e in ENGS}


def _attn_tables(rpb):
    kl = np.arange(128)
    krl, kc = kl // 64, kl % 64
    ql = np.arange(128)
    qrl, qc = ql // 64, ql % 64
    bias9 = np.zeros((128, 8, 9, 128), np.float32)
    for dt in range(-4, 5):
        dr = 2 * dt + krl[:, None] - qrl[None, :]
        dc = kc[:, None] - qc[None, :]
        ok = (np.abs(dr) <= 7) & (np.abs(dc) <= 15)
        ri = np.clip(dr + 7, 0, 14)
        ci = np.clip(dc + 15, 0, 30)
        g = rpb[:, ri, ci]
        bias9[:, :, dt + 4, :] = np.where(ok[None], g, 0.0).transpose(1, 0, 2)
    masks = np.full((128, 5, 5, 128), -1e30, np.float32)
    c0 = np.clip(qc - 8, 0, 48)
    colok = (kc[:, None] >= c0[None, :]) & (kc[:, None] < c0[None, :] + 16)
    for v, j in enumerate((2, 0, 1, 30, 31)):
        kt0 = min(max(j - 2, 0), 27)
        for i in range(5):
            kr = 2 * (kt0 + i) + krl
            qr = 2 * j + qrl
            r0 = np.clip(qr - 4, 0, 56)
            rowok = (kr[:, None] >= r0[None, :]) & (kr[:, None] < r0[None, :] + 8)
            masks[:, v, i, :] = np.where(rowok & colok, 0.0, -1e30)
    return bias9.reshape(128, 8, 9 * 128), masks.reshape(128, 5, 640)


def _variant(j):
    return {0: 1, 1: 2, 30: 3, 31: 4}.get(j, 0)


NMASK = 14
M_ID, M_TRIF, M_TRIB, M_NIF, M_NIB, M_STF, M_STB, M_BD32, M_OFF, M_BLK, M_CS0, M_CS1, M_COL, M_ONES = range(14)


def _dn_masks():
    j = np.arange(128)[:, None]
    i = np.arange(128)[None, :]
    same = (j // 64) == (i // 64)
    m = np.zeros((128, NMASK, 128), np.float32)
    m[:, M_ID] = (j == i)
    m[:, M_TRIF] = same & (j <= i)
    m[:, M_TRIB] = same & (j >= i)
    m[:, M_NIF] = np.where(same & (i >= j), 0.0, -1e30)
    m[:, M_NIB] = np.where(same & (i <= j), 0.0, -1e30)
    m[:, M_STF] = same & (i > j)
    m[:, M_STB] = same & (i < j)
    m[:, M_BD32] = (j // 32) == (i // 32)
    m[:, M_OFF] = same & ((j // 32) != (i // 32))
    m[:, M_BLK] = same
    m[:, M_CS0] = (j < 64) & (i >= 0)
    m[:, M_CS1] = (j >= 64) & (i >= 0)
    m[:, M_COL, 0] = (np.arange(128) < 64)
    m[:, M_COL, 1] = (np.arange(128) >= 64)
    m[:, M_COL, 2] = 1.0
    m[:, M_ONES] = 1.0
    return m


def _dn_block(S, E, rot, bf, cm, cmb, kq, Vt, Kt, g3, ball, nball, gcall, egt, edt, egs, S32d, WTzd, QTzd,
              psG, psR, psT, psA, psB, psU, psW, oacc, sbst, d, b, h, st, upto=6):
    P2 = [128, 128]
    ident_b = cmb[:, M_ID, :]
    col = lambda t: t[:, d, b, h:h + 1]
    E("pe", lambda e: e.matmul(psG[:], lhsT=kq[:, b, 0, :], rhs=kq[:, b].rearrange("p a t -> p (a t)"), start=True, stop=True),
      ["kq"], ["psG"])
    Gsb, Gn = rot("Gsb", [128, 256], F32, 2)
    E("act", lambda e: e.activation(out=Gsb[:], in_=psG[:], func=AF.Copy), ["psG"], [Gn])
    gb, gbn = rot("gb", [128, 3, 128], BF16, 2)
    for k3 in range(3):
        E("pool", lambda e, k3=k3: e.tensor_copy(out=gb[:, k3, :], in_=col(g3[k3]).to_broadcast(P2)), ["g3"], [gbn])
    for k3 in range(3):
        E("pe", lambda e, k3=k3: e.matmul(psR[:], lhsT=gb[:, k3, :], rhs=cmb[:, M_TRIF + d, :], start=(k3 == 0), stop=(k3 == 2)),
          [gbn, "cmb"], ["psR"], inc=(k3 == 2))
    egr, egrn = rot("egr", P2, F32, 2)
    E("act", lambda e: e.activation(out=egr[:], in_=psR[:], func=AF.Exp), ["psR"], [egrn])
    if upto <= 1:
        return
    t1, t1n = rot("t1", P2, F32, 2)
    E("dve", lambda e: e.scalar_tensor_tensor(out=t1[:], in0=psR[:], scalar=col(gcall), in1=cm[:, M_NIF + d, :],
                                              op0=ALU.subtract, op1=ALU.add), ["psR", "gcall", "cm"], [t1n])
    Di, Din = rot("Di", P2, F32, 2)
    E("act", lambda e: e.activation(out=Di[:], in_=t1[:], func=AF.Exp), [t1n], [Din])
    Ds, Dsn = rot("Ds", P2, F32, 2)
    E("pool", lambda e: e.tensor_tensor(out=Ds[:], in0=Di[:], in1=cm[:, M_STF + d, :], op=ALU.mult), [Din, "cm"], [Dsn])
    N0a, N0an = rot("N0a", P2, BF16, 2)
    E("dve", lambda e: e.scalar_tensor_tensor(out=N0a[:], in0=Gsb[:, 0:128], scalar=col(nball), in1=Ds[:], op0=ALU.mult, op1=ALU.mult),
      [Gn, "nball", Dsn], [N0an])
    QKm, QKmn = rot("QKm", P2, BF16, 2)
    E("pool", lambda e: e.tensor_tensor(out=QKm[:], in0=Gsb[:, 128:256], in1=Di[:], op=ALU.mult), [Gn, Din], [QKmn])
    NX, NXn = rot("NX", [128, 256], BF16, 3)
    E("pool", lambda e, NX=NX: e.tensor_tensor(out=NX[:, 0:128], in0=N0a[:], in1=cm[:, M_BD32, :], op=ALU.mult), [N0an, "cm"], [NXn])
    E("pe", lambda e: e.transpose(out=psT[:], in_=N0a[:], identity=ident_b), [N0an, "cmb"], ["psT"])
    P0a, P0an = rot("P0a", P2, BF16, 2)
    E("act", lambda e: e.activation(out=P0a[:], in_=psT[:], func=AF.Copy), ["psT"], [P0an])
    Pm, Pmn = rot("Pm", P2, BF16, 3)
    E("pool", lambda e, Pm=Pm: e.tensor_tensor(out=Pm[:], in0=P0a[:], in1=cm[:, M_BD32, :], op=ALU.mult), [P0an, "cm"], [Pmn])
    Pof, Pofn = rot("Pof", P2, BF16, 2)
    E("pool", lambda e: e.tensor_tensor(out=Pof[:], in0=P0a[:], in1=cm[:, M_OFF, :], op=ALU.mult), [P0an, "cm"], [Pofn])
    if upto <= 2:
        return
    X, Xn = rot("X", P2, F32, 2)
    E("dve", lambda e, NX=NX: e.tensor_tensor(out=X[:], in0=NX[:, 0:128], in1=cm[:, M_ID, :], op=ALU.add), [NXn, "cm"], [Xn])
    for m in range(4):
        pa, pan = psA[m % 2], "psA%d" % (m % 2)
        if m == 0:
            E("pe", lambda e, pa=pa, Pm=Pm, NX=NX: e.matmul(pa[:, 0:128], lhsT=Pm[:], rhs=NX[:, 0:128], start=True, stop=True),
              [Pmn, NXn], [pan])
        else:
            E("pe", lambda e, pa=pa, Pm=Pm, NX=NX: e.matmul(pa[:], lhsT=Pm[:], rhs=NX[:], start=True, stop=True), [Pmn, NXn], [pan])
        E("pe", lambda e, Pm=Pm, NX=NX: e.matmul(psB[:], lhsT=NX[:, 0:128], rhs=Pm[:], start=True, stop=True), [Pmn, NXn], ["psB"])
        NX2, NX2n = rot("NX", [128, 256], BF16, 3)
        E("act", lambda e, pa=pa, NX2=NX2: e.activation(out=NX2[:, 0:128], in_=pa[:, 0:128], func=AF.Copy), [pan], [NX2n])
        if m > 0:
            E("dve", lambda e, pa=pa: e.tensor_tensor(out=X[:], in0=X[:], in1=pa[:, 128:256], op=ALU.add), [Xn, pan], [Xn])
        E("pool", lambda e, NX2=NX2: e.tensor_copy(out=NX2[:, 128:256], in_=X[:]), [Xn], [NX2n])
        Pm2, Pm2n = rot("Pm", P2, BF16, 3)
        E("dve", lambda e, Pm2=Pm2: e.tensor_copy(out=Pm2[:], in_=psB[:]), ["psB"], [Pm2n])
        NX, NXn, Pm, Pmn = NX2, NX2n, Pm2, Pm2n
    E("pe", lambda e, Pm=Pm, NX=NX: e.matmul(psA[0][:, 0:128], lhsT=Pm[:], rhs=NX[:, 128:256], start=True, stop=True), [Pmn, NXn], ["psA0"])
    E("dve", lambda e: e.tensor_tensor(out=X[:], in0=X[:], in1=psA[0][:, 0:128], op=ALU.add), [Xn, "psA0"], [Xn])
    Xb, Xbn = rot("Xb", P2, BF16, 2)
    E("pool", lambda e: e.tensor_copy(out=Xb[:], in_=X[:]), [Xn], [Xbn])
    if upto <= 3:
        return
    E("pe", lambda e: e.transpose(out=psT[:], in_=Xb[:], identity=ident_b), [Xbn, "cmb"], ["psT"])
    Tb, Tbn = rot("Tb", P2, BF16, 2)
    E("act", lambda e: e.activation(out=Tb[:], in_=psT[:], func=AF.Copy), ["psT"], [Tbn])
    E("pe", lambda e: e.matmul(psB[:], lhsT=Pof[:], rhs=Xb[:], start=True, stop=True), [Pofn, Xbn], ["psB"])
    Y1, Y1n = rot("Y1", P2, BF16, 2)
    E("act", lambda e: e.activation(out=Y1[:], in_=psB[:], func=AF.Copy), ["psB"], [Y1n])
    E("pe", lambda e: e.matmul(psB[:], lhsT=Tb[:], rhs=Y1[:], start=True, stop=True), [Tbn, Y1n], ["psB"])
    Xf, Xfn = rot("Xf", P2, BF16, 2)
    E("dve", lambda e: e.tensor_tensor(out=Xf[:], in0=X[:], in1=psB[:], op=ALU.add), [Xn, "psB"], [Xfn])
    if upto <= 4:
        return
    Kg, Kgn = rot("Kg", P2, BF16, 2)
    E("pool", lambda e: e.tensor_scalar(out=Kg[:], in0=Kt[:, b, :], scalar1=col(egt), scalar2=None, op0=ALU.mult), ["Kt", "egt"], [Kgn])
    Kd, Kdn = rot("Kd", P2, BF16, 2)
    E("pool", lambda e: e.tensor_scalar(out=Kd[:], in0=Kt[:, b, :], scalar1=col(edt), scalar2=None, op0=ALU.mult), ["Kt", "edt"], [Kdn])
    E("pe", lambda e: e.matmul(psU[:, 0:128], lhsT=Xf[:], rhs=Vt[:, b, :], start=True, stop=True), [Xfn, "Vt"], ["psU"], inc=False)
    E("pe", lambda e: e.matmul(psU[:, 128:256], lhsT=Xf[:], rhs=Kg[:], start=True, stop=True), [Xfn, Kgn], ["psU"])
    bm, bmn = rot("bm", [128, 2], F32, 2)
    E("pool", lambda e: e.tensor_tensor(out=bm[:], in0=cm[:, M_COL, 0:2], in1=col(ball).to_broadcast([128, 2]), op=ALU.mult),
      ["cm", "ball"], [bmn])
    Um = []
    for c in range(2):
        u, un = rot("Um", P2, F32, 4)
        E("dve" if c == 0 else "act",
          (lambda e, u=u, c=c: e.tensor_scalar(out=u[:], in0=psU[:, 0:128], scalar1=bm[:, c:c + 1], scalar2=None, op0=ALU.mult)) if c == 0 else
          (lambda e, u=u, c=c: e.activation(out=u[:], in_=psU[:, 0:128], func=AF.Copy, scale=bm[:, c:c + 1])),
          ["psU", bmn], [un])
        Um.append((u, un))
    Wt, Wtn = rot("Wt", P2, BF16, 2)
    E("dve", lambda e: e.tensor_scalar(out=Wt[:], in0=psU[:, 128:256], scalar1=col(ball), scalar2=None, op0=ALU.mult), ["psU", "ball"], [Wtn])
    E("pe", lambda e: e.transpose(out=psT[:], in_=Wt[:], identity=ident_b), [Wtn, "cmb"], ["psT"])
    wn = ["WTz%d%d" % (d, c) for c in range(2)]
    qn = ["QTz%d%d" % (d, c) for c in range(2)]
    E("act", lambda e: e.activation(out=WTzd[0][:, 0:64], in_=psT[:, 0:64], func=AF.Copy), ["psT"], [wn[0]])
    E("dve", lambda e: e.tensor_copy(out=WTzd[1][:, 64:128], in_=psT[:, 64:128]), ["psT"], [wn[1]])
    for c in range(2):
        cs = slice(c * 64, (c + 1) * 64)
        E("pool", lambda e, c=c, cs=cs: e.tensor_tensor(out=QTzd[c][:, cs], in0=kq[:, b, 1, cs], in1=egr[:, cs], op=ALU.mult),
          ["kq", egrn], [qn[c]])
    if upto <= 5:
        return
    if st == 0:
        sb0, sb0n = rot("Sb%d" % d, P2, BF16, 3)
        E("pool", lambda e: e.memset(sb0[:], 0.0), [], [sb0n])
        sbst[d] = (sb0, sb0n)
    Sb, Sbn = sbst[d]
    s32n = "S32_%d" % d
    hist = []
    for c in ((0, 1) if d == 0 else (1, 0)):
        E("pe", lambda e, c=c, Sb=Sb: e.matmul(psW[:, 0, :], lhsT=WTzd[c][:], rhs=Sb[:], start=True, stop=True), [wn[c], Sbn], ["psW"])
        Vn, Vnn = rot("Vn", P2, BF16, 4)
        u, un = Um[c]
        E("dve", lambda e, Vn=Vn, u=u: e.tensor_tensor(out=Vn[:], in0=u[:], in1=psW[:, 0, :], op=ALU.subtract), [un, "psW"], [Vnn])
        E("pe", lambda e, Vn=Vn: e.matmul(psW[:, 1, :], lhsT=Kd[:], rhs=Vn[:], start=True, stop=True), [Kdn, Vnn], ["psW"])
        E("dve", lambda e, c=c: e.scalar_tensor_tensor(out=S32d[:], in0=S32d[:], scalar=egs[:, d, c, b, h:h + 1], in1=psW[:, 1, :],
                                                      op0=ALU.mult, op1=ALU.add), [s32n, "egs", "psW"], [s32n])
        hist.append((c, Sb, Sbn, Vn, Vnn))
        Sb, Sbn = rot("Sb%d" % d, P2, BF16, 3)
        E("act", lambda e, Sb=Sb: e.activation(out=Sb[:], in_=S32d[:], func=AF.Copy), [s32n], [Sbn])
    sbst[d] = (Sb, Sbn)
    k = 0
    for (c, Sbc, Sbcn, Vn, Vnn) in hist:
        E("pe", lambda e, c=c, Sbc=Sbc, k=k: e.matmul(psW[:, 2, :], lhsT=QTzd[c][:], rhs=Sbc[:], start=(k == 0), stop=False),
          [qn[c], Sbcn], ["psW"], inc=False)
        E("pe", lambda e, Vn=Vn, k=k: e.matmul(psW[:, 2, :], lhsT=QKm[:], rhs=Vn[:], start=False, stop=(k == 1)),
          [QKmn, Vnn], ["psW"], inc=(k == 1))
        k += 1
    E("dve", lambda e: e.tensor_tensor(out=oacc[:, b, :], in0=oacc[:, b, :], in1=psW[:, 2, :], op=ALU.add), ["oacc", "psW"], ["oacc"])


def _stage_c(nc, S, nseq, Bscr, scr, cin, dn_steps=32, dn_heads=4, debug=False, c_upto="T", blk_upto=6):
    lvl = "PGVRT".index(c_upto)
    dqkvT, zz, ab, mix = scr["dqkvT"], scr["zz"], scr["ab"], scr["mix"]
    dbg_o = nc.dram_tensor("dbg_o", [128, 32, 128], F32, kind="ExternalOutput").ap() if debug else None
    Bdbg = Buf("dbg_o")
    with ExitStack() as es:
        B = {}

        def bf(n):
            if n not in B:
                B[n] = Buf(n)
            return B[n]

        def sb(name, shape, dt):
            return es.enter_context(nc.sbuf_tensor("c_" + name, shape, dt))

        def ps(name, shape, dt):
            return es.enter_context(nc.psum_tensor("c_" + name, shape, dt))

        def E(eng, fn, r, w, inc=True):
            S.op(eng, fn, reads=[bf(n) for n in r], writes=[bf(n) for n in w], inc=inc)

        rots = {}

        def rot(name, shape, dt, n):
            if name not in rots:
                rots[name] = [[sb("%s%d" % (name, k), shape, dt) for k in range(n)], 0]
            lst, c = rots[name]
            rots[name][1] = c + 1
            return lst[c % n], "%s%d" % (name, c % n)

        cm = sb("cm", [128, NMASK, 128], F32)
        cmb = sb("cmb", [128, NMASK, 128], BF16)
        cw = sb("cw", [128, 12, 5], F32)
        dtb = sb("dtb", [128, 8], F32)
        alog = sb("alog", [128, 8], F32)
        negA = sb("negA", [128, 8], F32)
        gD = sb("gD", [128, 128], F32)
        epsc = sb("eps", [128, 1], F32)
        onec = sb("one", [128, 1], F32)
        raw = sb("raw", [128, T + 4], F32)
        acc = sb("acc", [128, T], F32)
        kq = sb("kq", [128, 32, 2, 128], BF16)
        vTb = sb("vTb", [128, T], BF16)
        Vt = sb("Vt", [128, 32, 128], BF16)
        Kt = sb("Kt", [128, 32, 128], BF16)
        abt = sb("abt", [128, 32, 16], F32)
        tmp8 = sb("tmp8", [128, 32, 8], F32)
        gall = sb("gall", [128, 2, 32, 4], F32)
        ball = sb("ball", [128, 2, 32, 4], F32)
        nball = sb("nball", [128, 2, 32, 4], F32)
        gcall = sb("gcall", [128, 2, 32, 4], F32)
        g3 = [sb("g3_%d" % k3, [128, 2, 32, 4], BF16) for k3 in range(3)]
        gres = sb("gres", [128, 2, 32, 4], F32)
        gso = sb("gso", [128, 2, 32, 4], F32)
        egt = sb("egt", [128, 2, 32, 4], F32)
        edt = sb("edt", [128, 2, 32, 4], F32)
        egs = sb("egs", [128, 2, 2, 32, 4], F32)
        oacc = sb("oacc", [128, 32, 128], F32)
        zt = sb("zt", [128, 32, 128], F32)
        ssq = sb("ssq", [128, 32], F32)
        S32 = [sb("S32_%d" % d, [128, 128], F32) for d in range(2)]
        WTz = [[sb("WTz%d%d" % (d, c), [128, 128], BF16) for c in range(2)] for d in range(2)]
        QTz = [[sb("QTz%d%d" % (d, c), [128, 128], BF16) for c in range(2)] for d in range(2)]

        psG = ps("G", [128, 512], F32)[:, 0:256]
        psR = ps("R", [128, 512], F32)[:, 0:128]
        psT = ps("T", [128, 1024], BF16)[:, 0:128]
        psA = [ps("A%d" % k, [128, 512], F32)[:, 0:256] for k in range(2)]
        psB = ps("B", [128, 512], F32)[:, 0:128]
        psU = ps("U", [128, 512], F32)[:, 0:256]
        psW = ps("W", [128, 4, 128], F32)

        ident_b = cmb[:, M_ID, :]
        S.dma("sp", lambda e: e.dma_start(out=cm[:], in_=cin["cm"]), writes=[bf("cm")])
        S.dma("pool", lambda e: e.dma_start(out=cmb[:], in_=cin["cm"]), writes=[bf("cmb")])
        S.dma("sp", lambda e: e.dma_start(out=cw[:], in_=cin["cw"]), writes=[bf("cw")])
        S.dma("sp", lambda e: e.dma_start(out=dtb[:], in_=cin["dtb"]), writes=[bf("dtb")])
        S.dma("sp", lambda e: e.dma_start(out=alog[:], in_=cin["alog"]), writes=[bf("alog")])
        S.dma("sp", lambda e: e.dma_start(out=gD[:], in_=cin["gDb"]), writes=[bf("gD")])
        E("pool", lambda e: e.memset(epsc[:], EPS), [], ["eps"])
        E("pool", lambda e: e.memset(onec[:], 1.0), [], ["one"])
        E("pool", lambda e: e.memset(raw[:, 0:2], 0.0), [], ["raw"])
        E("pool", lambda e: e.memset(raw[:, T + 2:T + 4], 0.0), [], ["raw"])
        for d in range(2):
            for c in range(2):
                E("pool", lambda e, d=d, c=c: e.memset(WTz[d][c][:], 0.0), [], ["WTz%d%d" % (d, c)])
                E("pool", lambda e, d=d, c=c: e.memset(QTz[d][c][:], 0.0), [], ["QTz%d%d" % (d, c)])
        E("act", lambda e: e.activation(out=negA[:], in_=alog[:], func=AF.Exp), ["alog"], ["negA"])
        E("dve", lambda e: e.tensor_scalar(out=negA[:], in0=negA[:], scalar1=-1.0, scalar2=None, op0=ALU.mult), ["negA"], ["negA"])

        for s in range(nseq if lvl >= 1 else 0):
            for pc8 in range(8):
                S.dma("sp", lambda e, s=s, pc8=pc8: e.dma_start(
                    out=abt[:, pc8 * 4:(pc8 + 1) * 4, :],
                    in_=ab[s, pc8 * 512:(pc8 + 1) * 512, :].rearrange("(b p) c -> p b c", p=128)),
                    reads=[Bscr["ab"]], writes=[bf("abt")])
            E("dve", lambda e: e.tensor_tensor(out=tmp8[:], in0=abt[:, :, 0:8], in1=dtb[:].unsqueeze(1).to_broadcast([128, 32, 8]),
                                               op=ALU.add), ["abt", "dtb"], ["tmp8"])
            E("act", lambda e: e.activation(out=tmp8[:], in_=tmp8[:], func=AF.Exp), ["tmp8"], ["tmp8"])
            E("act", lambda e: e.activation(out=tmp8[:], in_=tmp8[:], func=AF.Ln, bias=onec[:], scale=1.0), ["tmp8", "one"], ["tmp8"])
            E("dve", lambda e: e.tensor_tensor(out=gall[:].rearrange("p d b h -> p b d h"),
                                               in0=tmp8[:].rearrange("p b (d h) -> p b d h", d=2),
                                               in1=negA[:].rearrange("p (d h) -> p d h", d=2).unsqueeze(1).to_broadcast([128, 32, 2, 4]),
                                               op=ALU.mult), ["tmp8", "negA"], ["gall"])
            E("act", lambda e: e.activation(out=tmp8[:], in_=abt[:, :, 8:16], func=AF.Exp, scale=-1.0), ["abt"], ["tmp8"])
            E("dve", lambda e: e.tensor_scalar(out=tmp8[:], in0=tmp8[:], scalar1=1.0, scalar2=None, op0=ALU.add), ["tmp8"], ["tmp8"])
            E("dve", lambda e: e.reciprocal(out=ball[:].rearrange("p d b h -> p b d h"),
                                            in_=tmp8[:].rearrange("p b (d h) -> p b d h", d=2)), ["tmp8"], ["ball"])
            E("dve", lambda e: e.tensor_scalar(out=nball[:], in0=ball[:], scalar1=-1.0, scalar2=None, op0=ALU.mult), ["ball"], ["nball"])
            E("dve", lambda e: e.tensor_copy(out=g3[0][:], in_=gall[:]), ["gall"], ["g3"])
            E("dve", lambda e: e.tensor_tensor(out=gres[:], in0=gall[:], in1=g3[0][:], op=ALU.subtract), ["gall", "g3"], ["gres"])
            E("dve", lambda e: e.tensor_copy(out=g3[1][:], in_=gres[:]), ["gres"], ["g3"])
            E("dve", lambda e: e.tensor_tensor(out=gres[:], in0=gres[:], in1=g3[1][:], op=ALU.subtract), ["gres", "g3"], ["gres"])
            E("dve", lambda e: e.tensor_copy(out=g3[2][:], in_=gres[:]), ["gres"], ["g3"])

            def mm3(mask_idx, d):
                for k3 in range(3):
                    E("pe", lambda e, k3=k3: e.matmul(psR[:], lhsT=cmb[:, mask_idx, :], rhs=g3[k3][:, d].rearrange("p b h -> p (b h)"),
                                                      start=(k3 == 0), stop=(k3 == 2)), ["g3", "cmb"], ["psR"], inc=(k3 == 2))
            for d in range(2):
                mm3(M_TRIF + d, d)
                E("act", lambda e, d=d: e.activation(out=gcall[:, d].rearrange("p b h -> p (b h)"), in_=psR[:], func=AF.Copy), ["psR"], ["gcall"])
                mm3(M_BLK, d)
                E("act", lambda e, d=d: e.activation(out=gso[:, d].rearrange("p b h -> p (b h)"), in_=psR[:], func=AF.Copy), ["psR"], ["gso"])
                for c in range(2):
                    mm3(M_CS0 + c, d)
                    E("act", lambda e, d=d, c=c: e.activation(out=egs[:, d, c].rearrange("p b h -> p (b h)"), in_=psR[:], func=AF.Exp),
                      ["psR"], ["egs"])
            E("act", lambda e: e.activation(out=egt[:], in_=gcall[:], func=AF.Exp), ["gcall"], ["egt"])
            E("dve", lambda e: e.tensor_tensor(out=edt[:], in0=gso[:], in1=gcall[:], op=ALU.subtract), ["gso", "gcall"], ["edt"])
            E("act", lambda e: e.activation(out=edt[:], in_=edt[:], func=AF.Exp), ["edt"], ["edt"])

            for h in range(dn_heads if lvl >= 2 else 0):
                for qi, chunk in enumerate((h, 4 + h, 8 + h)):
                    S.dma("sp", lambda e, s=s, chunk=chunk: e.dma_start(out=raw[:, 2:T + 2], in_=dqkvT[s, chunk]),
                          reads=[Bscr["dqkvT"]], writes=[bf("raw")])
                    for half in range(2):
                        eng = "dve"
                        c0 = half * 2048
                        E(eng, lambda e, c0=c0, chunk=chunk: e.tensor_scalar(
                            out=acc[:, c0:c0 + 2048], in0=raw[:, c0:c0 + 2048], scalar1=cw[:, chunk, 0:1], scalar2=None, op0=ALU.mult),
                          ["raw", "cw"], ["acc%d" % half])
                        for tap in range(1, 5):
                            E(eng, lambda e, c0=c0, chunk=chunk, tap=tap: e.scalar_tensor_tensor(
                                out=acc[:, c0:c0 + 2048], in0=raw[:, c0 + tap:c0 + tap + 2048], scalar=cw[:, chunk, tap:tap + 1],
                                in1=acc[:, c0:c0 + 2048], op0=ALU.mult, op1=ALU.add), ["raw", "cw", "acc%d" % half], ["acc%d" % half])
                    if qi == 2:
                        E("act", lambda e: e.activation(out=vTb[:], in_=acc[:], func=AF.Silu), ["acc0", "acc1"], ["vTb"])
                        for b in range(32):
                            E("pe", lambda e, b=b: e.transpose(out=psT[:], in_=vTb[:, b * 128:(b + 1) * 128], identity=ident_b),
                              ["vTb", "cmb"], ["psT"])
                            E("dve" if b % 2 else "act",
                              (lambda e, b=b: e.tensor_copy(out=Vt[:, b, :], in_=psT[:])) if b % 2 else
                              (lambda e, b=b: e.activation(out=Vt[:, b, :], in_=psT[:], func=AF.Copy)), ["psT"], ["Vt"])
                    else:
                        E("act", lambda e: e.activation(out=acc[:], in_=acc[:], func=AF.Silu), ["acc0", "acc1"], ["acc0", "acc1"])
                        for pc in range(8):
                            sqt, sqn = rot("sq", [128, 512], BF16, 2)
                            rrt, rrn = rot("rr", [128, 512], F32, 2)
                            cs = slice(pc * 512, (pc + 1) * 512)
                            E("act", lambda e, sqt=sqt, cs=cs: e.activation(out=sqt[:], in_=acc[:, cs], func=AF.Square), ["acc0", "acc1"], [sqn])
                            for hf in range(2):
                                E("pe", lambda e, sqt=sqt, hf=hf: e.matmul(psA[hf][:], lhsT=cmb[:, M_ONES, :],
                                                                         rhs=sqt[:, hf * 256:(hf + 1) * 256], start=True, stop=True),
                                  [sqn, "cmb"], ["psA%d" % hf])
                                E("act", lambda e, rrt=rrt, hf=hf: e.activation(out=rrt[:, hf * 256:(hf + 1) * 256], in_=psA[hf][:],
                                                                                func=AF.Sqrt, bias=epsc[:], scale=1.0),
                                  ["psA%d" % hf, "eps"], [rrn])
                            E("dve", lambda e, rrt=rrt: e.reciprocal(out=rrt[:], in_=rrt[:]), [rrn], [rrn])
                            sc = (128.0 ** -0.5) if qi == 0 else 1.0
                            slot = 1 if qi == 0 else 0
                            E("dve", lambda e, rrt=rrt, cs=cs, pc=pc, sc=sc, slot=slot: e.scalar_tensor_tensor(
                                out=kq[:, pc * 4:(pc + 1) * 4, slot, :], in0=acc[:, cs].rearrange("p (b t) -> p b t", t=128), scalar=sc,
                                in1=rrt[:].rearrange("p (b t) -> p b t", t=128), op0=ALU.mult, op1=ALU.mult),
                              ["acc0", "acc1", rrn], ["kq"])
                for b in range(32):
                    E("pe", lambda e, b=b: e.transpose(out=psT[:], in_=kq[:, b, 0, :], identity=ident_b), ["kq", "cmb"], ["psT"])
                    E("dve" if b % 2 else "act",
                      (lambda e, b=b: e.tensor_copy(out=Kt[:, b, :], in_=psT[:])) if b % 2 else
                      (lambda e, b=b: e.activation(out=Kt[:, b, :], in_=psT[:], func=AF.Copy)), ["psT"], ["Kt"])
                for d in range(2):
                    E("pool", lambda e, d=d: e.memset(S32[d][:], 0.0), [], ["S32_%d" % d])
                E("pool", lambda e: e.memset(oacc[:], 0.0), [], ["oacc"])
                sbst = {}
                for st in range(dn_steps if lvl >= 3 else 0):
                    for d in range(2):
                        b = st if d == 0 else 31 - st
                        _dn_block(S, E, rot, bf, cm, cmb, kq, Vt, Kt, g3, ball, nball, gcall, egt, edt, egs, S32[d], WTz[d], QTz[d],
                                  psG, psR, psT, psA, psB, psU, psW, oacc, sbst, d, b, h, st, blk_upto)
                if debug and s == 0 and h == 0:
                    for pc8 in range(8):
                        S.dma("sp", lambda e, pc8=pc8: e.dma_start(out=dbg_o[:, pc8 * 4:(pc8 + 1) * 4, :], in_=oacc[:, pc8 * 4:(pc8 + 1) * 4, :]),
                              reads=[bf("oacc")], writes=[Bdbg], sem_buf=bf("oacc"))
                if lvl < 4:
                    continue
                for pc8 in range(8):
                    S.dma("sp", lambda e, s=s, h=h, pc8=pc8: e.dma_start(
                        out=zt[:, pc8 * 4:(pc8 + 1) * 4, :],
                        in_=zz[s, pc8 * 512:(pc8 + 1) * 512, h * 128:(h + 1) * 128].rearrange("(b p) c -> p b c", p=128)),
                        reads=[Bscr["zz"]], writes=[bf("zt")])
                E("act", lambda e: e.activation(out=zt[:], in_=zt[:], func=AF.Silu), ["zt"], ["zt"])
                tq, tqn = rot("big", [128, 32, 128], F32, 1)
                E("pool", lambda e, tq=tq: e.tensor_tensor(out=tq[:], in0=oacc[:], in1=oacc[:], op=ALU.mult), ["oacc"], [tqn])
                E("dve", lambda e, tq=tq: e.tensor_reduce(out=ssq[:], in_=tq[:], axis=AX.X, op=ALU.add), [tqn], ["ssq"])
                E("act", lambda e: e.activation(out=ssq[:], in_=ssq[:], func=AF.Sqrt, bias=epsc[:], scale=1.0 / 128), ["ssq", "eps"], ["ssq"])
                E("dve", lambda e: e.reciprocal(out=ssq[:], in_=ssq[:]), ["ssq"], ["ssq"])
                E("dve", lambda e: e.tensor_tensor(out=oacc[:], in0=oacc[:], in1=ssq[:].unsqueeze(2).to_broadcast([128, 32, 128]), op=ALU.mult),
                  ["oacc", "ssq"], ["oacc"])
                E("pool", lambda e: e.tensor_tensor(out=oacc[:], in0=oacc[:], in1=gD[:].unsqueeze(1).to_broadcast([128, 32, 128]), op=ALU.mult),
                  ["oacc", "gD"], ["oacc"])
                E("dve", lambda e, tq=tq: e.tensor_tensor(out=tq[:], in0=oacc[:], in1=zt[:], op=ALU.mult), ["oacc", "zt"], [tqn])
                for pc8 in range(8):
                    r0 = s * T + pc8 * 512
                    S.dma("sp", lambda e, tq=tq, r0=r0, h=h, pc8=pc8: e.dma_start(
                        out=mix[r0:r0 + 512, 512 + h * 128:512 + (h + 1) * 128].rearrange("(b p) c -> p b c", p=128),
                        in_=tq[:, pc8 * 4:(pc8 + 1) * 4, :]),
                        reads=[bf(tqn)], writes=[Bscr["mix"]], sem_buf=bf(tqn))
        S.wait_all("sp", [Bscr["mix"], Bdbg])
        S.barrier()
        S.emit()

def _stage_d(nc, S, nseq, Bscr, x, mix, din_d, scr_d, ntiles=None):
    NT = nseq * T
    ntiles = NT // 128 if ntiles is None else ntiles
    with ExitStack() as es:
        B = {}

        def bf(n):
            if n not in B:
                B[n] = Buf(n)
            return B[n]

        def sb(name, shape, dt):
            return es.enter_context(nc.sbuf_tensor("d_" + name, shape, dt))

        def ps(name, shape, dt):
            return es.enter_context(nc.psum_tensor("d_" + name, shape, dt))

        def E(eng, fn, r, w, inc=True):
            S.op(eng, fn, reads=[bf(n) for n in r], writes=[bf(n) for n in w], inc=inc)
        wo = sb("wo", [128, 8, D], BF16)
        wr = sb("wr", [128, 8, 72], F32)
        wrh = sb("wrh", [128, 8, 72], BF16)
        wrl = sb("wrl", [128, 8, 72], BF16)
        wres = sb("wres", [128, 8, 72], F32)
        g2 = sb("g2", [128, D], F32)
        rb = sb("rb", [128, 72], F32)
        idb = sb("idb", [128, 128], BF16)
        epsc = sb("eps", [128, 1], F32)
        mt = [sb("mt%d" % i, [128, D], F32) for i in range(2)]
        xt = [sb("xt%d" % i, [128, D], F32) for i in range(2)]
        mb = sb("mb", [128, D], BF16)
        mT = sb("mT", [128, 8, 128], BF16)
        x1 = [sb("x1_%d" % i, [128, D], F32) for i in range(2)]
        junk = sb("junk", [128, D], F32)
        ss = sb("ss", [128, 1], F32)
        h2 = sb("h2", [128, D], F32)
        hh = sb("hh", [128, D], BF16)
        hl = sb("hl", [128, D], BF16)
        hres = sb("hres", [128, D], F32)
        hT = [sb("hT%d" % i, [128, 2, 8, 128], BF16) for i in range(2)]
        lg = sb("lg", [128, 72], F32)
        w8 = [sb("w8_%d" % i, [128, 8], F32) for i in range(6)]
        c1 = [sb("c1_%d" % i, [128, 1], F32) for i in range(8)]
        t64 = sb("t64", [128, 8, 8], F32)
        G = [sb("G%d" % i, [128, 8, 8], F32) for i in range(2)]
        ptr = [ps("ptr%d" % i, [128, 1024], BF16) for i in range(2)]
        po = [ps("po%d" % i, [128, 512], F32) for i in range(2)]
        pl = ps("pl", [128, 512], F32)

        for kc in range(8):
            S.dma("pool", lambda e, kc=kc: e.dma_start(out=wo[:, kc, :], in_=din_d["w_out"][kc * 128:(kc + 1) * 128, :]), writes=[bf("wo")])
        S.dma("sp", lambda e: e.dma_start(out=wr[:], in_=din_d["wr"].rearrange("(k p) c -> p k c", p=128)), writes=[bf("wr")])
        S.dma("sp", lambda e: e.dma_start(out=g2[:], in_=din_d["g2b"]), writes=[bf("g2")])
        S.dma("sp", lambda e: e.dma_start(out=rb[:], in_=din_d["rbb"]), writes=[bf("rb")])
        S.dma("pool", lambda e: e.dma_start(out=idb[:], in_=din_d["ident"]), writes=[bf("idb")])
        E("pool", lambda e: e.memset(epsc[:], EPS), [], ["eps"])
        E("dve", lambda e: e.tensor_copy(out=wrh[:], in_=wr[:]), ["wr"], ["wrh"])
        E("dve", lambda e: e.tensor_tensor(out=wres[:], in0=wr[:], in1=wrh[:], op=ALU.subtract), ["wr", "wrh"], ["wres"])
        E("dve", lambda e: e.tensor_copy(out=wrl[:], in_=wres[:]), ["wres"], ["wrl"])

        for ti in range(ntiles):
            r0 = ti * 128
            i2 = ti % 2
            MT, XT, X1, HT, GG = mt[i2], xt[i2], x1[i2], hT[i2], G[i2]
            S.dma("sp", lambda e, MT=MT, r0=r0: e.dma_start(out=MT[:], in_=mix[r0:r0 + 128, :]), reads=[Bscr["mix"]], writes=[bf("mt%d" % i2)])
            S.dma("sp", lambda e, XT=XT, r0=r0: e.dma_start(out=XT[:], in_=x[r0:r0 + 128, :]), writes=[bf("xt%d" % i2)])
            E("dve", lambda e, MT=MT: e.tensor_copy(out=mb[:], in_=MT[:]), ["mt%d" % i2], ["mb"])
            for half in range(2):
                for k4 in range(4):
                    kc = half * 4 + k4
                    E("pe", lambda e, half=half, k4=k4, kc=kc: e.transpose(out=ptr[half][:, k4 * 128:(k4 + 1) * 128],
                                                                          in_=mb[:, kc * 128:(kc + 1) * 128], identity=idb[:]),
                      ["mb", "idb"], ["ptr%d" % half], inc=(k4 == 3))
                E("act" if half else "dve",
                  (lambda e, half=half: e.activation(out=mT[:, half * 4:(half + 1) * 4, :].rearrange("p k t -> p (k t)"), in_=ptr[half][:, 0:512], func=AF.Copy)) if half else
                  (lambda e, half=half: e.tensor_copy(out=mT[:, half * 4:(half + 1) * 4, :].rearrange("p k t -> p (k t)"), in_=ptr[half][:, 0:512])),
                  ["ptr%d" % half], ["mT"])
            for ch in range(2):
                for kc in range(8):
                    E("pe", lambda e, ch=ch, kc=kc: e.matmul(po[ch][:], lhsT=mT[:, kc, :], rhs=wo[:, kc, ch * 512:(ch + 1) * 512],
                                                            start=(kc == 0), stop=(kc == 7)), ["mT", "wo"], ["po%d" % ch], inc=(kc == 7))
                E("dve", lambda e, ch=ch, X1=X1, XT=XT: e.tensor_tensor(out=X1[:, ch * 512:(ch + 1) * 512], in0=XT[:, ch * 512:(ch + 1) * 512],
                                                                     in1=po[ch][:], op=ALU.add), ["xt%d" % i2, "po%d" % ch], ["x1_%d" % i2])
            S.dma("sp", lambda e, X1=X1, r0=r0: e.dma_start(out=scr_d["x1"][r0:r0 + 128, :], in_=X1[:]),
                  reads=[bf("x1_%d" % i2)], writes=[Bscr["x1"]], sem_buf=bf("x1_%d" % i2))
            E("act", lambda e, X1=X1: e.activation(out=junk[:], in_=X1[:], func=AF.Square, accum_out=ss[:]), ["x1_%d" % i2], ["junk", "ss"])
            E("act", lambda e: e.activation(out=ss[:], in_=ss[:], func=AF.Sqrt, bias=epsc[:], scale=1.0 / D), ["ss", "eps"], ["ss"])
            E("dve", lambda e: e.reciprocal(out=ss[:], in_=ss[:]), ["ss"], ["ss"])
            E("dve", lambda e, X1=X1: e.scalar_tensor_tensor(out=h2[:], in0=X1[:], scalar=ss[:], in1=g2[:], op0=ALU.mult, op1=ALU.mult),
              ["x1_%d" % i2, "ss", "g2"], ["h2"])
            E("dve", lambda e: e.tensor_copy(out=hh[:], in_=h2[:]), ["h2"], ["hh"])
            E("dve", lambda e: e.tensor_tensor(out=hres[:], in0=h2[:], in1=hh[:], op=ALU.subtract), ["h2", "hh"], ["hres"])
            E("dve", lambda e: e.tensor_copy(out=hl[:], in_=hres[:]), ["hres"], ["hl"])
            for part, src in ((0, hh), (1, hl)):
                for half in range(2):
                    for k4 in range(4):
                        kc = half * 4 + k4
                        E("pe", lambda e, half=half, k4=k4, kc=kc, src=src: e.transpose(
                            out=ptr[half][:, k4 * 128:(k4 + 1) * 128], in_=src[:, kc * 128:(kc + 1) * 128], identity=idb[:]),
                          ["hh", "hl", "idb"], ["ptr%d" % half], inc=(k4 == 3))
                    E("act" if half else "dve",
                      (lambda e, half=half, part=part, HT=HT: e.activation(out=HT[:, part, half * 4:(half + 1) * 4, :].rearrange("p k t -> p (k t)"),
                                                                           in_=ptr[half][:, 0:512], func=AF.Copy)) if half else
                      (lambda e, half=half, part=part, HT=HT: e.tensor_copy(out=HT[:, part, half * 4:(half + 1) * 4, :].rearrange("p k t -> p (k t)"),
                                                                            in_=ptr[half][:, 0:512])),
                      ["ptr%d" % half], ["hT%d" % i2])
            S.dma("sp", lambda e, HT=HT, r0=r0: e.dma_start(
                out=scr_d["h2T"][:, :, r0:r0 + 128], in_=HT[:, 0, :, :]),
                reads=[bf("hT%d" % i2)], writes=[Bscr["h2T"]], sem_buf=bf("hT%d" % i2))
            terms = [(0, wrh), (0, wrl), (1, wrh)]
            n = 0
            for part, wpart in terms:
                for kc in range(8):
                    E("pe", lambda e, part=part, wpart=wpart, kc=kc, n=n, HT=HT: e.matmul(
                        pl[:, 0:72], lhsT=HT[:, part, kc, :], rhs=wpart[:, kc, :], start=(n == 0), stop=(n == 23)),
                      ["hT%d" % i2, "wrh", "wrl"], ["pl"], inc=(n == 23))
                    n += 1
            E("dve", lambda e: e.tensor_tensor(out=lg[:], in0=pl[:, 0:72], in1=rb[:], op=ALU.add), ["pl", "rb"], ["lg"])
            gl = lg[:, 0:8]
            el = lg[:, 8:72].rearrange("p (g e) -> p g e", e=8)
            gmax, gsum, m1, m2, gp, wa = c1[0], c1[1], c1[2], c1[3], c1[4], c1[5]
            ohg, eg_, ing, oh1, msk, oh2 = w8
            E("dve", lambda e: e.tensor_reduce(out=gmax[:], in_=gl, axis=AX.X, op=ALU.max), ["lg"], ["gmax"])
            E("dve", lambda e: e.tensor_scalar(out=ohg[:], in0=gl, scalar1=gmax[:], scalar2=None, op0=ALU.is_equal), ["lg", "gmax"], ["ohg"])
            E("dve", lambda e: e.tensor_scalar(out=eg_[:], in0=gl, scalar1=gmax[:], scalar2=None, op0=ALU.subtract), ["lg", "gmax"], ["eg"])
            E("act", lambda e: e.activation(out=eg_[:], in_=eg_[:], func=AF.Exp, accum_out=gsum[:]), ["eg"], ["eg", "gsum"])
            E("dve", lambda e: e.reciprocal(out=gp[:], in_=gsum[:]), ["gsum"], ["gp"])
            E("dve", lambda e: e.tensor_tensor(out=t64[:], in0=el, in1=ohg[:].unsqueeze(2).to_broadcast([128, 8, 8]), op=ALU.mult), ["lg", "ohg"], ["t64"])
            E("dve", lambda e: e.tensor_reduce(out=ing[:], in_=t64[:].rearrange("p g e -> p e g"), axis=AX.X, op=ALU.add), ["t64"], ["ing"])
            E("dve", lambda e: e.tensor_reduce(out=m1[:], in_=ing[:], axis=AX.X, op=ALU.max), ["ing"], ["m1"])
            E("dve", lambda e: e.tensor_scalar(out=oh1[:], in0=ing[:], scalar1=m1[:], scalar2=None, op0=ALU.is_equal), ["ing", "m1"], ["oh1"])
            E("dve", lambda e: e.scalar_tensor_tensor(out=msk[:], in0=oh1[:], scalar=-1e30, in1=ing[:], op0=ALU.mult, op1=ALU.add), ["oh1", "ing"], ["msk"])
            E("dve", lambda e: e.tensor_reduce(out=m2[:], in_=msk[:], axis=AX.X, op=ALU.max), ["msk"], ["m2"])
            E("dve", lambda e: e.tensor_scalar(out=oh2[:], in0=msk[:], scalar1=m2[:], scalar2=None, op0=ALU.is_equal), ["msk", "m2"], ["oh2"])
            E("dve", lambda e: e.tensor_tensor(out=wa[:], in0=m2[:], in1=m1[:], op=ALU.subtract), ["m1", "m2"], ["wa"])
            E("act", lambda e: e.activation(out=wa[:], in_=wa[:], func=AF.Exp), ["wa"], ["wa"])
            E("dve", lambda e: e.tensor_scalar(out=wa[:], in0=wa[:], scalar1=1.0, scalar2=None, op0=ALU.add), ["wa"], ["wa"])
            E("dve", lambda e: e.reciprocal(out=wa[:], in_=wa[:]), ["wa"], ["wa"])
            ga, gb_ = c1[6], c1[7]
            E("dve", lambda e: e.tensor_tensor(out=ga[:], in0=wa[:], in1=gp[:], op=ALU.mult), ["wa", "gp"], ["ga"])
            E("dve", lambda e: e.tensor_tensor(out=gb_[:], in0=gp[:], in1=ga[:], op=ALU.subtract), ["gp", "ga"], ["gb"])
            E("dve", lambda e: e.tensor_scalar(out=oh1[:], in0=oh1[:], scalar1=ga[:], scalar2=None, op0=ALU.mult), ["oh1", "ga"], ["oh1"])
            E("dve", lambda e: e.scalar_tensor_tensor(out=oh1[:], in0=oh2[:], scalar=gb_[:], in1=oh1[:], op0=ALU.mult, op1=ALU.add),
              ["oh2", "gb", "oh1"], ["oh1"])
            E("dve", lambda e, GG=GG: e.tensor_tensor(out=GG[:], in0=ohg[:].unsqueeze(2).to_broadcast([128, 8, 8]),
                                                     in1=oh1[:].unsqueeze(1).to_broadcast([128, 8, 8]), op=ALU.mult), ["ohg", "oh1"], ["G%d" % i2])
            S.dma("sp", lambda e, GG=GG, r0=r0: e.dma_start(out=scr_d["G"][r0:r0 + 128, :], in_=GG[:].rearrange("p g e -> p (g e)")),
                  reads=[bf("G%d" % i2)], writes=[Bscr["G"]], sem_buf=bf("G%d" % i2))
        S.wait_all("sp", [Bscr["x1"], Bscr["h2T"], Bscr["G"]])
        S.barrier()
        S.emit()


def build_d_only(ntok, ntiles):
    nc = bass.Bass("TRN2", target_bir_lowering=False)
    din = lambda name, shape: nc.dram_tensor(name, shape, F32, kind="ExternalInput").ap()
    x = din("x", [ntok, D]); mix = din("mix", [ntok, D])
    din_d = dict(w_out=din("w_out", [D, D]), wr=din("wr", [D, 72]), g2b=din("g2b", [128, D]), rbb=din("rbb", [128, 72]), ident=din("ident", [128, 128]))
    scr_d = dict(x1=nc.dram_tensor("x1", [ntok, D], F32, kind="ExternalOutput").ap(),
                 h2T=nc.dram_tensor("h2T", [128, 8, ntok], BF16, kind="ExternalOutput").ap(),
                 G=nc.dram_tensor("G", [ntok, 64], F32, kind="ExternalOutput").ap())
    Bscr = {n: Buf(n) for n in ("mix", "x1", "h2T", "G")}
    with ExitStack() as ges:
        S = Sched(nc, ges)
        _stage_d(nc, S, 1, Bscr, x, mix, din_d, scr_d, ntiles=ntiles)
    return nc


def _stage_e(nc, S, Bscr, scr_d, wE, out, ntok, experts=range(64), TG=512):
    experts = list(experts)
    with ExitStack() as es:
        B = {}

        def bf(n):
            if n not in B:
                B[n] = Buf(n)
            return B[n]

        def sb(name, shape, dt):
            return es.enter_context(nc.sbuf_tensor("e_" + name, shape, dt))

        def ps(name, shape, dt):
            return es.enter_context(nc.psum_tensor("e_" + name, shape, dt))

        def E(eng, fn, r, w, inc=True):
            S.op(eng, fn, reads=[bf(n) for n in r], writes=[bf(n) for n in w], inc=inc)
        NTL = TG // 128
        hT = sb("hT", [128, 8, TG], BF16)
        Gt = sb("Gt", [128, NTL, 64], F32)
        yacc = sb("yacc", [128, NTL, D], F32)
        wg = [sb("wg%d" % i, [128, 8, 256], BF16) for i in range(2)]
        wu = [sb("wu%d" % i, [128, 8, 256], BF16) for i in range(2)]
        wd = [sb("wd%d" % i, [128, 2, D], BF16) for i in range(2)]
        sg = [sb("sg%d" % i, [128, 512], F32) for i in range(2)]
        hid = [sb("hid%d" % i, [128, 2, TG], BF16) for i in range(2)]
        pg = [ps("pg%d" % i, [128, 512], F32) for i in range(2)]
        pu = [ps("pu%d" % i, [128, 512], F32) for i in range(2)]
        py = [ps("py%d" % i, [128, 512], F32) for i in range(2)]
        for g0 in range(0, ntok, TG):
            S.dma("sp", lambda e, g0=g0: e.dma_start(out=hT[:], in_=scr_d["h2T"][:, :, g0:g0 + TG]), reads=[Bscr["h2T"]], writes=[bf("hT")])
            S.dma("sp", lambda e, g0=g0: e.dma_start(out=Gt[:], in_=scr_d["G"][g0:g0 + TG, :].rearrange("(t p) c -> p t c", p=128)),
                  reads=[Bscr["G"]], writes=[bf("Gt")])
            S.dma("sp", lambda e, g0=g0: e.dma_start(out=yacc[:], in_=scr_d["x1"][g0:g0 + TG, :].rearrange("(t p) c -> p t c", p=128)),
                  reads=[Bscr["x1"]], writes=[bf("yacc")])
            for ie, ex in enumerate(experts):
                w2 = ie % 2
                S.dma("pool", lambda e, ex=ex, w2=w2: e.dma_start(out=wg[w2][:], in_=wE["w_gate"][ex].rearrange("(k p) c -> p k c", p=128)), writes=[bf("wg%d" % w2)])
                S.dma("pool", lambda e, ex=ex, w2=w2: e.dma_start(out=wu[w2][:], in_=wE["w_up"][ex].rearrange("(k p) c -> p k c", p=128)), writes=[bf("wu%d" % w2)])
                S.dma("pool", lambda e, ex=ex, w2=w2: e.dma_start(out=wd[w2][:], in_=wE["w_down"][ex].rearrange("(k p) c -> p k c", p=128)), writes=[bf("wd%d" % w2)])
                H = hid[w2]
                NSUB = TG // 512
                for c in range(2):
                    for sub in range(NSUB):
                        k2 = (c * NSUB + sub) % 2
                        ts_ = slice(sub * 512, (sub + 1) * 512)
                        for kc in range(8):
                            E("pe", lambda e, c=c, kc=kc, w2=w2, k2=k2, ts_=ts_: e.matmul(
                                pg[k2][:], lhsT=wg[w2][:, kc, c * 128:(c + 1) * 128], rhs=hT[:, kc, ts_],
                                start=(kc == 0), stop=(kc == 7)), ["wg%d" % w2, "hT"], ["pg%d" % k2], inc=(kc == 7))
                        for kc in range(8):
                            E("pe", lambda e, c=c, kc=kc, w2=w2, k2=k2, ts_=ts_: e.matmul(
                                pu[k2][:], lhsT=wu[w2][:, kc, c * 128:(c + 1) * 128], rhs=hT[:, kc, ts_],
                                start=(kc == 0), stop=(kc == 7)), ["wu%d" % w2, "hT"], ["pu%d" % k2], inc=(kc == 7))
                        E("act", lambda e, k2=k2: e.activation(out=sg[k2][:], in_=pg[k2][:], func=AF.Silu), ["pg%d" % k2], ["sg%d" % k2])
                        E("dve", lambda e, c=c, H=H, k2=k2, ts_=ts_: e.tensor_tensor(out=H[:, c, ts_], in0=sg[k2][:], in1=pu[k2][:], op=ALU.mult),
                          ["sg%d" % k2, "pu%d" % k2], ["hid%d" % w2])
                for t in range(NTL):
                    for half in range(2):
                        p2 = (t * 2 + half) % 2
                        for c in range(2):
                            E("pe", lambda e, t=t, half=half, c=c, p2=p2, H=H, w2=w2: e.matmul(
                                py[p2][:], lhsT=H[:, c, t * 128:(t + 1) * 128], rhs=wd[w2][:, c, half * 512:(half + 1) * 512],
                                start=(c == 0), stop=(c == 1)), ["hid%d" % w2, "wd%d" % w2], ["py%d" % p2], inc=(c == 1))
                        E("dve", lambda e, t=t, half=half, p2=p2, ex=ex: e.scalar_tensor_tensor(
                            out=yacc[:, t, half * 512:(half + 1) * 512], in0=py[p2][:], scalar=Gt[:, t, ex:ex + 1],
                            in1=yacc[:, t, half * 512:(half + 1) * 512], op0=ALU.mult, op1=ALU.add), ["py%d" % p2, "Gt", "yacc"], ["yacc"])
            S.dma("sp", lambda e, g0=g0: e.dma_start(out=out[g0:g0 + TG, :].rearrange("(t p) c -> p t c", p=128), in_=yacc[:]),
                  reads=[bf("yacc")], writes=[Bscr["out"]], sem_buf=bf("yacc"))
        S.wait_all("sp", [Bscr["out"]])
        S.barrier()
        S.emit()


def build_e_only(ntok, experts):
    nc = bass.Bass("TRN2", target_bir_lowering=False)
    scr_d = dict(x1=nc.dram_tensor("x1", [ntok, D], F32, kind="ExternalInput").ap(),
                 h2T=nc.dram_tensor("h2T", [128, 8, ntok], BF16, kind="ExternalInput").ap(),
                 G=nc.dram_tensor("G", [ntok, 64], F32, kind="ExternalInput").ap())
    wE = dict(w_gate=nc.dram_tensor("w_gate", [64, D, 256], F32, kind="ExternalInput").ap(),
              w_up=nc.dram_tensor("w_up", [64, D, 256], F32, kind="ExternalInput").ap(),
              w_down=nc.dram_tensor("w_down", [64, 256, D], F32, kind="ExternalInput").ap())
    out = nc.dram_tensor("out", [ntok, D], F32, kind="ExternalOutput").ap()
    Bscr = {n: Buf(n) for n in ("x1", "h2T", "G", "out")}
    with ExitStack() as ges:
        S = Sched(nc, ges)
        _stage_e(nc, S, Bscr, scr_d, wE, out, ntok, experts=experts, TG=ntok)
    return nc


def build_c_only(nseq=1, dn_steps=32, dn_heads=4, c_upto="T", blk_upto=6, debug=True):
    nc = bass.Bass("TRN2", target_bir_lowering=False)
    NT = nseq * T
    din = lambda name, shape: nc.dram_tensor(name, shape, F32, kind="ExternalInput").ap()
    scr = dict(dqkvT=din("dqkvT", [nseq, 12, 128, T]), zz=din("zz", [nseq, T, 512]), ab=din("ab", [nseq, T, 16]),
               mix=nc.dram_tensor("mix", [NT, D], F32, kind="ExternalOutput").ap())
    cin = dict(cw=din("cw", [128, 12, 5]), cm=din("cm", [128, NMASK, 128]), dtb=din("dtb", [128, 8]),
               alog=din("alog", [128, 8]), gDb=din("gDb", [128, 128]))
    Bscr = {n: Buf(n) for n in ("dqkvT", "zz", "ab", "mix")}
    with ExitStack() as ges:
        S = Sched(nc, ges)
        _stage_c(nc, S, nseq, Bscr, scr, cin, dn_steps, dn_heads, debug, c_upto, blk_upto)
    return nc


def build(nseq, debug=False, stages="ABCDE", dn_steps=32, dn_heads=4, c_upto="T", blk_upto=6, e_experts=range(64)):
    nc = bass.Bass("TRN2", target_bir_lowering=False)
    NT = nseq * T
    okind = "ExternalOutput" if debug else "Internal"

    def din(name, shape, dt=F32):
        return nc.dram_tensor(name, shape, dt, kind="ExternalInput").ap()

    x = din("x", [NT, D])
    w_in = din("w_in", [D, INC])
    g1b = din("g1b", [128, D])
    gq2 = din("gq2", [128, 1])
    gk2 = din("gk2", [128, 1])
    bones = din("bones", [128, 128])
    ident = din("ident", [128, 128])
    bias9 = din("bias9", [128, 8, 1152])
    amask = din("amask", [128, 5, 640])
    gAb = din("gAb", [128, 512])
    cin = dict(cw=din("cw", [128, 12, 5]), cm=din("cm", [128, NMASK, 128]), dtb=din("dtb", [128, 8]),
               alog=din("alog", [128, 8]), gDb=din("gDb", [128, 128]))

    qT = nc.dram_tensor("qT", [nseq, 4, 128, T], BF16, kind=okind).ap()
    kT = nc.dram_tensor("kT", [nseq, 4, 128, T], BF16, kind=okind).ap()
    vp = nc.dram_tensor("vp", [nseq, T, 520], BF16, kind=okind).ap()
    dqkvT = nc.dram_tensor("dqkvT", [nseq, 12, 128, T], F32, kind=okind).ap()
    zz = nc.dram_tensor("zz", [nseq, T, 512], F32, kind=okind).ap()
    ab = nc.dram_tensor("ab", [nseq, T, 16], F32, kind=okind).ap()
    mix = nc.dram_tensor("mix", [NT, D], F32, kind=okind).ap()
    full = ("D" in stages) and ("E" in stages)
    if full:
        din_d = dict(w_out=din("w_out", [D, D]), wr=din("wr", [D, 72]), g2b=din("g2b", [128, D]), rbb=din("rbb", [128, 72]), ident=ident)
        wE = dict(w_gate=din("w_gate", [64, D, 256]), w_up=din("w_up", [64, D, 256]), w_down=din("w_down", [64, 256, D]))
        scr_d = dict(x1=nc.dram_tensor("x1", [NT, D], F32, kind=okind).ap(),
                     h2T=nc.dram_tensor("h2T", [128, 8, NT], BF16, kind=okind).ap(),
                     G=nc.dram_tensor("G", [NT, 64], F32, kind=okind).ap())
        out = nc.dram_tensor("out", [NT, D], F32, kind="ExternalOutput").ap()

    Bscr = {n: Buf(n) for n in ("qT", "kT", "vp", "dqkvT", "zz", "ab", "mix", "x1", "h2T", "G", "out")}

    with ExitStack() as ges:
        S = Sched(nc, ges)

        with ExitStack() as es:
            def sb(name, shape, dt):
                return es.enter_context(nc.sbuf_tensor(name, shape, dt))

            def ps(name, shape, dt):
                return es.enter_context(nc.psum_tensor(name, shape, dt))
            wsb = sb("wsb", [128, 8, INC], BF16)
            g1 = sb("g1", [128, D], F32)
            gq = sb("gq", [128, 1], F32)
            gk = sb("gk", [128, 1], F32)
            gq8 = sb("gq8", [128, 1], F32)
            gk8 = sb("gk8", [128, 1], F32)
            bo = sb("bo", [128, 128], BF16)
            idb = sb("idb", [128, 128], BF16)
            epsc = sb("epsc", [128, 1], F32)
            xg = [sb("xg%d" % i, [128, 4, D], F32) for i in range(2)]
            junk = sb("junk", [128, D], F32)
            ss = sb("ss", [128, 4], F32)
            rstd = sb("rstd", [128, 4], F32)
            hb = sb("hb", [128, 4, D], BF16)
            hT = [sb("hT%d" % i, [128, 8, 512], BF16) for i in range(2)]
            sq = [sb("sq%d" % i, [128, 512], BF16) for i in range(2)]
            rr = [sb("rr%d" % i, [128, 512], F32) for i in range(2)]
            oqk = [sb("oqk%d" % i, [128, 512], BF16) for i in range(3)]
            of = [sb("of%d" % i, [128, 512], F32) for i in range(3)]
            vt = [sb("vt%d" % i, [128, 8, 65], BF16) for i in range(2)]
            oab = [sb("oab%d" % i, [128, 16], F32) for i in range(2)]
            ptr = [ps("ptr%d" % i, [128, 512], BF16) for i in range(2)]
            pf = [ps("pf%d" % i, [128, 512], F32) for i in range(3)]
            pn = [ps("pn%d" % i, [128, 512], F32) for i in range(2)]

            B = {}

            def bf(n):
                if n not in B:
                    B[n] = Buf(n)
                return B[n]

            for kc in range(8):
                S.dma("pool", lambda e, kc=kc: e.dma_start(out=wsb[:, kc, :], in_=w_in[kc * 128:(kc + 1) * 128, :]),
                      writes=[bf("w%d" % kc)])
            S.dma("sp", lambda e: e.dma_start(out=g1[:], in_=g1b), writes=[bf("g1")])
            S.dma("sp", lambda e: e.dma_start(out=gq[:], in_=gq2), writes=[bf("gq")])
            S.dma("sp", lambda e: e.dma_start(out=gk[:], in_=gk2), writes=[bf("gk")])
            S.dma("pool", lambda e: e.dma_start(out=bo[:], in_=bones), writes=[bf("bo")])
            S.dma("pool", lambda e: e.dma_start(out=idb[:], in_=ident), writes=[bf("idb")])
            S.op("pool", lambda e: e.memset(epsc[:], EPS), writes=[bf("eps")])
            S.op("dve", lambda e: e.tensor_scalar(out=gq8[:], in0=gq[:], scalar1=0.125, scalar2=None, op0=ALU.mult),
                 reads=[bf("gq")], writes=[bf("gq8")])
            S.op("dve", lambda e: e.tensor_scalar(out=gk8[:], in0=gk[:], scalar1=8.0, scalar2=None, op0=ALU.mult),
                 reads=[bf("gk")], writes=[bf("gk8")])
            for i in range(2):
                S.op("pool", lambda e, i=i: e.memset(vt[i][:, :, 64:65], 1.0), writes=[bf("vt%d" % i)])
            wall = [bf("w%d" % kc) for kc in range(8)]

            cnt = {"pf": 0, "pn": 0, "sq": 0, "oqk": 0, "of": 0, "vt": 0, "oab": 0, "ptr": 0}

            def rot(name, n):
                i = cnt[name] % n
                cnt[name] += 1
                return i

            for s in range(nseq):
                for g in range(8):
                    gi = s * 8 + g
                    tok0 = s * T + g * 512
                    X, BX = xg[gi % 2], bf("xg%d" % (gi % 2))
                    H, BH = hT[gi % 2], bf("hT%d" % (gi % 2))
                    S.dma("sp", lambda e, X=X, tok0=tok0: e.dma_start(
                        out=X[:], in_=x[tok0:tok0 + 512, :].rearrange("(t p) d -> p t d", p=128)), writes=[BX])
                    for t in range(4):
                        S.op("act", lambda e, X=X, t=t: e.activation(out=junk[:], in_=X[:, t, :], func=AF.Square,
                                                                      accum_out=ss[:, t:t + 1]),
                             reads=[BX], writes=[bf("junk"), bf("ss")])
                    S.op("act", lambda e: e.activation(out=rstd[:], in_=ss[:], func=AF.Sqrt, bias=epsc[:], scale=1.0 / D),
                         reads=[bf("ss"), bf("eps")], writes=[bf("rstd")])
                    S.op("dve", lambda e: e.reciprocal(out=rstd[:], in_=rstd[:]), reads=[bf("rstd")], writes=[bf("rstd")])
                    for t in range(4):
                        S.op("dve", lambda e, X=X, t=t: e.scalar_tensor_tensor(
                            out=hb[:, t, :], in0=X[:, t, :], scalar=rstd[:, t:t + 1], in1=g1[:], op0=ALU.mult, op1=ALU.mult),
                            reads=[BX, bf("rstd"), bf("g1")], writes=[bf("hb")])
                    for kc in range(8):
                        pi = rot("ptr", 2)
                        for t in range(4):
                            S.op("pe", lambda e, pi=pi, t=t, kc=kc: e.transpose(
                                out=ptr[pi][:, t * 128:(t + 1) * 128], in_=hb[:, t, kc * 128:(kc + 1) * 128], identity=idb[:]),
                                reads=[bf("hb"), bf("idb")], writes=[bf("ptr%d" % pi)], inc=(t == 3))
                        S.op("act" if kc % 2 else "dve",
                             (lambda e, pi=pi, kc=kc, H=H: e.activation(out=H[:, kc, :], in_=ptr[pi][:], func=AF.Copy)) if kc % 2 else
                             (lambda e, pi=pi, kc=kc, H=H: e.tensor_copy(out=H[:, kc, :], in_=ptr[pi][:])),
                             reads=[bf("ptr%d" % pi)], writes=[BH])
                    for c in list(range(8)) + list(range(12, 24)):
                        pi = rot("pf", 3)
                        for kc in range(8):
                            S.op("pe", lambda e, pi=pi, kc=kc, c=c, H=H: e.matmul(
                                pf[pi][:], lhsT=wsb[:, kc, c * 128:(c + 1) * 128], rhs=H[:, kc, :], start=(kc == 0), stop=(kc == 7)),
                                reads=[BH] + (wall if kc == 0 else []), writes=[bf("pf%d" % pi)], inc=(kc == 7))
                        if c < 8:
                            si, ni, oi = rot("sq", 2), rot("pn", 2), rot("oqk", 3)
                            S.op("act", lambda e, pi=pi, si=si: e.activation(out=sq[si][:], in_=pf[pi][:], func=AF.Square),
                                 reads=[bf("pf%d" % pi)], writes=[bf("sq%d" % si)])
                            S.op("pe", lambda e, ni=ni, si=si: e.matmul(pn[ni][:], lhsT=bo[:], rhs=sq[si][:], start=True, stop=True),
                                 reads=[bf("sq%d" % si), bf("bo")], writes=[bf("pn%d" % ni)])
                            S.op("act", lambda e, ni=ni: e.activation(out=rr[ni][:], in_=pn[ni][:], func=AF.Sqrt,
                                                                       bias=epsc[:], scale=1.0 / 64),
                                 reads=[bf("pn%d" % ni), bf("eps")], writes=[bf("rr%d" % ni)])
                            S.op("dve", lambda e, ni=ni: e.reciprocal(out=rr[ni][:], in_=rr[ni][:]),
                                 reads=[bf("rr%d" % ni)], writes=[bf("rr%d" % ni)])
                            gcol = gq8 if c < 4 else gk
                            S.op("dve", lambda e, pi=pi, ni=ni, oi=oi, gcol=gcol: e.scalar_tensor_tensor(
                                out=oqk[oi][:], in0=pf[pi][:], scalar=gcol[:], in1=rr[ni][:], op0=ALU.mult, op1=ALU.mult),
                                reads=[bf("pf%d" % pi), bf("rr%d" % ni), bf("gq8"), bf("gk")], writes=[bf("oqk%d" % oi)])
                            dst = (qT if c < 4 else kT)[s, c % 4, :, g * 512:(g + 1) * 512]
                            S.dma("sp", lambda e, oi=oi, dst=dst: e.dma_start(out=dst, in_=oqk[oi][:]),
                                  reads=[bf("oqk%d" % oi)], writes=[Bscr["qT" if c < 4 else "kT"]], sem_buf=bf("oqk%d" % oi))
                        else:
                            oi = rot("of", 3)
                            S.op("act", lambda e, pi=pi, oi=oi: e.activation(out=of[oi][:], in_=pf[pi][:], func=AF.Copy),
                                 reads=[bf("pf%d" % pi)], writes=[bf("of%d" % oi)])
                            dst = dqkvT[s, c - 12, :, g * 512:(g + 1) * 512]
                            S.dma("sp", lambda e, oi=oi, dst=dst: e.dma_start(out=dst, in_=of[oi][:]),
                                  reads=[bf("of%d" % oi)], writes=[Bscr["dqkvT"]], sem_buf=bf("of%d" % oi))
                    for t in range(4):
                        r0 = g * 512 + t * 128
                        for which, c0, ncol in (("v", 1024, 512), ("z", 3072, 512), ("ab", 3584, 16)):
                            pi = rot("pf", 3)
                            for kc in range(8):
                                S.op("pe", lambda e, pi=pi, kc=kc, t=t, c0=c0, ncol=ncol, H=H: e.matmul(
                                    pf[pi][:, 0:ncol], lhsT=H[:, kc, t * 128:(t + 1) * 128], rhs=wsb[:, kc, c0:c0 + ncol],
                                    start=(kc == 0), stop=(kc == 7)),
                                    reads=[BH], writes=[bf("pf%d" % pi)], inc=(kc == 7))
                            if which == "v":
                                oi = rot("vt", 2)
                                S.op("dve", lambda e, pi=pi, oi=oi: e.tensor_copy(
                                    out=vt[oi][:, :, 0:64], in_=pf[pi][:].rearrange("p (h d) -> p h d", d=64)),
                                    reads=[bf("pf%d" % pi)], writes=[bf("vt%d" % oi)])
                                S.dma("sp", lambda e, oi=oi, r0=r0, s=s: e.dma_start(
                                    out=vp[s, r0:r0 + 128, :], in_=vt[oi][:].rearrange("p h e -> p (h e)")),
                                    reads=[bf("vt%d" % oi)], writes=[Bscr["vp"]], sem_buf=bf("vt%d" % oi))
                            elif which == "z":
                                oi = rot("of", 3)
                                S.op("act", lambda e, pi=pi, oi=oi: e.activation(out=of[oi][:], in_=pf[pi][:], func=AF.Copy),
                                     reads=[bf("pf%d" % pi)], writes=[bf("of%d" % oi)])
                                S.dma("sp", lambda e, oi=oi, r0=r0, s=s: e.dma_start(out=zz[s, r0:r0 + 128, :], in_=of[oi][:]),
                                      reads=[bf("of%d" % oi)], writes=[Bscr["zz"]], sem_buf=bf("of%d" % oi))
                            else:
                                oi = rot("oab", 2)
                                S.op("dve", lambda e, pi=pi, oi=oi: e.tensor_copy(out=oab[oi][:], in_=pf[pi][:, 0:16]),
                                     reads=[bf("pf%d" % pi)], writes=[bf("oab%d" % oi)])
                                S.dma("sp", lambda e, oi=oi, r0=r0, s=s: e.dma_start(out=ab[s, r0:r0 + 128, :], in_=oab[oi][:]),
                                      reads=[bf("oab%d" % oi)], writes=[Bscr["ab"]], sem_buf=bf("oab%d" % oi))
            S.wait_all("sp", list(Bscr.values()))
            S.emit()

        S.barrier()
        with ExitStack() as es:
            def sb(name, shape, dt):
                return es.enter_context(nc.sbuf_tensor(name, shape, dt))

            def ps(name, shape, dt):
                return es.enter_context(nc.psum_tensor(name, shape, dt))
            qs = sb("qs", [128, 4, T], BF16)
            ks = sb("ks", [128, 4, T], BF16)
            vs = sb("vs", [128, 32, 520], BF16)
            b9 = sb("b9", [128, 8, 1152], F32)
            am = sb("am", [128, 5, 640], F32)
            gA = sb("gA", [128, 512], F32)
            epsc = sb("epsc2", [128, 1], F32)
            t1 = [sb("t1_%d" % i, [128, 640], F32) for i in range(2)]
            t2 = [sb("t2_%d" % i, [128, 640], F32) for i in range(2)]
            pT = [sb("pT%d" % i, [128, 640], BF16) for i in range(2)]
            rden = sb("rden", [128, 2, 4], F32)
            oo = sb("oo", [128, 512], F32)
            junk = sb("junk2", [128, 512], F32)
            s1 = sb("s1", [128, 1], F32)
            om = [sb("om%d" % i, [128, 512], F32) for i in range(2)]
            st = [ps("st%d" % i, [128, 1024], F32) for i in range(2)]
            po = [ps("po%d" % i, [128, 2, 512], F32) for i in range(2)]
            B = {}

            def bf(n):
                if n not in B:
                    B[n] = Buf(n)
                return B[n]
            S.dma("sp", lambda e: e.dma_start(out=b9[:], in_=bias9), writes=[bf("b9")])
            S.dma("sp", lambda e: e.dma_start(out=am[:], in_=amask), writes=[bf("am")])
            S.dma("sp", lambda e: e.dma_start(out=gA[:], in_=gAb), writes=[bf("gA")])
            S.op("pool", lambda e: e.memset(epsc[:], EPS), writes=[bf("eps")])
            it = 0
            for s in range(nseq):
                for c in range(4):
                    S.dma("sp", lambda e, c=c, s=s: e.dma_start(out=qs[:, c, :], in_=qT[s, c]), reads=[Bscr["qT"]], writes=[bf("qs")])
                    S.dma("sp", lambda e, c=c, s=s: e.dma_start(out=ks[:, c, :], in_=kT[s, c]), reads=[Bscr["kT"]], writes=[bf("ks")])
                for q4 in range(4):
                    S.dma("sp", lambda e, q4=q4, s=s: e.dma_start(
                        out=vs[:, q4 * 8:(q4 + 1) * 8, :],
                        in_=vp[s, q4 * 1024:(q4 + 1) * 1024, :].rearrange("(t p) e -> p t e", p=128)),
                        reads=[Bscr["vp"]], writes=[bf("vs")])
                for j in range(32):
                    kt0 = min(max(j - 2, 0), 27)
                    v = _variant(j)
                    d0 = kt0 - j + 4
                    PO, BPO = po[j % 2], bf("po%d" % (j % 2))
                    for h in range(8):
                        hp, p0 = h // 2, (h % 2) * 64
                        i2 = it % 2
                        it += 1
                        ST, BST = st[i2], bf("st%d" % i2)
                        for i in range(5):
                            S.op("pe", lambda e, ST=ST, i=i, hp=hp, p0=p0, kt0=kt0, j=j: e.matmul(
                                ST[:, i * 128:(i + 1) * 128],
                                lhsT=ks[p0:p0 + 64, hp, (kt0 + i) * 128:(kt0 + i + 1) * 128],
                                rhs=qs[p0:p0 + 64, hp, j * 128:(j + 1) * 128], start=True, stop=True),
                                reads=[bf("qs"), bf("ks")], writes=[BST], inc=(i == 4))
                        S.op("dve", lambda e, ST=ST, i2=i2, h=h, d0=d0: e.tensor_tensor(
                            out=t1[i2][:], in0=ST[:, 0:640], in1=b9[:, h, d0 * 128:d0 * 128 + 640], op=ALU.add),
                            reads=[BST, bf("b9")], writes=[bf("t1_%d" % i2)])
                        S.op("pool", lambda e, i2=i2, v=v: e.tensor_tensor(
                            out=t2[i2][:], in0=t1[i2][:], in1=am[:, v, :], op=ALU.add),
                            reads=[bf("t1_%d" % i2), bf("am")], writes=[bf("t2_%d" % i2)])
                        S.op("act", lambda e, i2=i2: e.activation(out=pT[i2][:], in_=t2[i2][:], func=AF.Exp),
                             reads=[bf("t2_%d" % i2)], writes=[bf("pT%d" % i2)])
                        for i in range(5):
                            S.op("pe", lambda e, PO=PO, i=i, h=h, i2=i2, kt0=kt0: e.matmul(
                                PO[:, h // 4, (h % 4) * 65:(h % 4) * 65 + 65],
                                lhsT=pT[i2][:, i * 128:(i + 1) * 128], rhs=vs[:, kt0 + i, h * 65:(h + 1) * 65],
                                start=(i == 0), stop=(i == 4)),
                                reads=[bf("pT%d" % i2), bf("vs")], writes=[BPO], inc=(i == 4))
                    pov = PO[:, :, 0:260].rearrange("p a (h e) -> p a h e", e=65)
                    S.op("dve", lambda e, pov=pov: e.reciprocal(out=rden[:], in_=pov[:, :, :, 64]),
                         reads=[BPO], writes=[bf("rden")])
                    S.op("dve", lambda e, pov=pov: e.tensor_tensor(
                        out=oo[:].rearrange("p (a h d) -> p a h d", a=2, h=4), in0=pov[:, :, :, 0:64],
                        in1=rden[:].unsqueeze(3).to_broadcast([128, 2, 4, 64]), op=ALU.mult),
                        reads=[BPO, bf("rden")], writes=[bf("oo")])
                    S.op("act", lambda e: e.activation(out=junk[:], in_=oo[:], func=AF.Square, accum_out=s1[:]),
                         reads=[bf("oo")], writes=[bf("junk"), bf("s1")])
                    S.op("act", lambda e: e.activation(out=s1[:], in_=s1[:], func=AF.Sqrt, bias=epsc[:], scale=1.0 / 512),
                         reads=[bf("s1"), bf("eps")], writes=[bf("s1")])
                    S.op("dve", lambda e: e.reciprocal(out=s1[:], in_=s1[:]), reads=[bf("s1")], writes=[bf("s1")])
                    OM, BOM = om[j % 2], bf("om%d" % (j % 2))
                    S.op("dve", lambda e, OM=OM: e.scalar_tensor_tensor(
                        out=OM[:], in0=oo[:], scalar=s1[:], in1=gA[:], op0=ALU.mult, op1=ALU.mult),
                        reads=[bf("oo"), bf("s1"), bf("gA")], writes=[BOM])
                    r0 = s * T + j * 128
                    S.dma("sp", lambda e, OM=OM, r0=r0: e.dma_start(out=mix[r0:r0 + 128, 0:512], in_=OM[:]),
                          reads=[BOM], writes=[Bscr["mix"]], sem_buf=BOM)
            S.wait_all("sp", [Bscr["mix"]])
            S.emit()

        if "C" in stages:
            S.barrier()
            _stage_c(nc, S, nseq, Bscr, dict(dqkvT=dqkvT, zz=zz, ab=ab, mix=mix), cin, dn_steps, dn_heads, debug, c_upto, blk_upto)
        if full:
            S.barrier()
            _stage_d(nc, S, nseq, Bscr, x, mix, din_d, scr_d)
            S.barrier()
            _stage_e(nc, S, Bscr, scr_d, wE, out, NT, experts=e_experts, TG=2048)
    return nc


def _prep(inputs):
    f = lambda a: np.ascontiguousarray(np.asarray(a, dtype=np.float32))
    c = {}
    c["w_in"] = f(inputs["w_in"][0])
    c["g1b"] = f(np.broadcast_to(inputs["ln1_g"][0][None, :], (128, D)))
    c["gq2"] = f(np.tile(inputs["attn_q_norm_g"][0], 2)[:, None])
    c["gk2"] = f(np.tile(inputs["attn_k_norm_g"][0], 2)[:, None])
    p = np.arange(128)
    c["bones"] = f((p[:, None] // 64) == (p[None, :] // 64))
    c["ident"] = f(np.eye(128))
    b9, am = _attn_tables(np.asarray(inputs["attn_rpb"][0], np.float32))
    c["bias9"], c["amask"] = f(b9), f(am)
    c["gAb"] = f(np.broadcast_to(inputs["attn_out_norm_g"][0][None, :], (128, 512)))
    c["cw"] = f(np.asarray(inputs["dn_conv_w"][0]).T.reshape(12, 128, 5).transpose(1, 0, 2))
    c["cm"] = f(_dn_masks())
    c["dtb"] = f(np.broadcast_to(np.asarray(inputs["dn_dt_bias"][0]).reshape(1, 8), (128, 8)))
    c["alog"] = f(np.broadcast_to(np.asarray(inputs["dn_a_log"][0]).reshape(1, 8), (128, 8)))
    c["gDb"] = f(np.broadcast_to(np.asarray(inputs["dn_out_norm_g"][0])[None, :], (128, 128)))
    return c


def _prep_d(inputs):
    f = lambda a: np.ascontiguousarray(np.asarray(a, dtype=np.float32))
    c = {}
    c["w_out"] = f(inputs["w_out"][0])
    c["wr"] = f(np.concatenate([inputs["router_group_w"][0], inputs["router_expert_w"][0]], axis=1))
    c["g2b"] = f(np.broadcast_to(inputs["ln2_g"][0][None, :], (128, D)))
    c["rbb"] = f(np.broadcast_to(np.concatenate([inputs["router_group_b"][0], inputs["router_expert_b"][0]])[None, :], (128, 72)))
    c["ident"] = f(np.eye(128))
    return c


def _in_maps(inputs, n=8, nseq=2):
    x = np.asarray(inputs["x"], np.float32).reshape(16 * T, D)
    consts = dict(_prep(inputs))
    consts.update(_prep_d(inputs))
    f = lambda a: np.ascontiguousarray(np.asarray(a, dtype=np.float32))
    consts["w_gate"] = f(inputs["expert_w_gate"][0])
    consts["w_up"] = f(inputs["expert_w_up"][0])
    consts["w_down"] = f(inputs["expert_w_down"][0])
    maps = []
    for i in range(n):
        m = dict(consts)
        m["x"] = np.ascontiguousarray(x[i * nseq * T:(i + 1) * nseq * T])
        maps.append(m)
    return maps


def kernel(**inputs):
    nc = build(2)
    res = run_bass_kernel_spmd(nc, _in_maps(inputs), core_ids=list(range(8)))
    out = np.concatenate([np.asarray(r["out"]) for r in res.results], axis=0)
    return np.ascontiguousarray(out.reshape(16, T, D).astype(np.float32))
```
